# Optimizing a Trainium2 kernel written in Bass

```python
import math
import jax, jax.numpy as jnp
from jax import lax
import numpy as np

D_MODEL = 1024
BATCH = 4
SEQ = 8192
DEPTH = 4
DEC_BATCH = 16
DEC_SEQ = 2048
PAST_LEN = 128

MIX_WIDTH = 1024
HG_HEADS = 5
HG_DK = 128
HG_DV = 128
HG_WIDTH = 640
HG_CHUNK = 64
ATTN_WINDOWS = (128, 512, 2048)
ATTN_DILATIONS = (1, 4, 16)
ATTN_GROUPS = 3
ATTN_HEADS_PER_GROUP = 2
ATTN_HEADS = 6
HEAD_DIM = 64
ATTN_WIDTH = 384
ROT_DIM = 16
ROPE_THETA = 500000.0
IN_COLS = 5 * 640 + 3 * 384
N_EXPERTS = 16
EXPERT_FF = 1024
EC_CAPACITY = 2
NORM_EPS = 1e-6

kernel_name = "hybrid_hgrn2_dilated_attn_ec_moe_encoder"


def rms_norm(x, w):
    xf = x.astype(jnp.float32)
    y = xf * lax.rsqrt(jnp.mean(xf * xf, axis=-1, keepdims=True) + NORM_EPS)
    return (y * w.astype(jnp.float32)).astype(x.dtype)


def partial_rotary(t, pos):
    half = ROT_DIM // 2
    inv_freq = ROPE_THETA ** (-jnp.arange(half, dtype=jnp.float32) * 2.0 / ROT_DIM)
    ang = pos.astype(jnp.float32)[:, None] * inv_freq[None, :]
    cos = jnp.cos(ang)[None, :, None, :]
    sin = jnp.sin(ang)[None, :, None, :]
    t1 = t[..., :half]
    t2 = t[..., half:ROT_DIM]
    return jnp.concatenate([t1 * cos - t2 * sin, t1 * sin + t2 * cos, t[..., ROT_DIM:]], axis=-1)


def layer_lower_bounds(logits):
    c = jnp.cumsum(jax.nn.softmax(logits.astype(jnp.float32), axis=0), axis=0)
    return c - c[0:1]


def gla_chunk_scan(q, k, v, logf):
    B, S, H, DK = q.shape
    DV = v.shape[-1]
    C = HG_CHUNK
    n = S // C

    def to_chunks(t):
        return t.reshape(B, n, C, H, t.shape[-1]).transpose(1, 0, 3, 2, 4)

    tril = jnp.tril(jnp.ones((C, C), dtype=bool))[:, :, None]

    def step(state, inp):
        q_c, k_c, v_c, g_c = inp
        b = jnp.cumsum(g_c, axis=2)
        diff = b[:, :, :, None, :] - b[:, :, None, :, :]
        decay = jnp.exp(jnp.where(tril, diff, -jnp.inf))
        scores = jnp.sum(q_c[:, :, :, None, :] * k_c[:, :, None, :, :] * decay, axis=-1)
        o = (jnp.einsum('bhts,bhse->bhte', scores, v_c)
             + jnp.einsum('bhtd,bhde->bhte', q_c * jnp.exp(b), state))
        b_last = b[:, :, -1:, :]
        new_state = (jnp.exp(b_last[:, :, 0, :])[..., None] * state
                     + jnp.einsum('bhsd,bhse->bhde', k_c * jnp.exp(b_last - b), v_c))
        return new_state, o

    init = jnp.zeros((B, H, DK, DV), jnp.float32)
    _, o = lax.scan(step, init, (to_chunks(q), to_chunks(k), to_chunks(v), to_chunks(logf)))
    return o.transpose(1, 0, 3, 2, 4).reshape(B, S, H, DV)


def hgrn2_mixer(q_raw, zf, zb, i_raw, g_raw, lb_f, lb_b, norm_w):
    B, S, _ = q_raw.shape
    dt = q_raw.dtype

    def heads(t):
        return t.astype(jnp.float32).reshape(B, S, HG_HEADS, HG_DK)

    q = jax.nn.silu(heads(q_raw))
    v = heads(i_raw)

    def log_forget(z, lb):
        lb = lb.reshape(HG_HEADS, HG_DK)
        return jnp.logaddexp(jnp.log(lb), jnp.log1p(-lb) + jax.nn.log_sigmoid(z))

    logf_f = log_forget(heads(zf), lb_f)
    logf_b = log_forget(heads(zb), lb_b)
    k_f = -jnp.expm1(logf_f)
    k_b = -jnp.expm1(logf_b)

    o_f = gla_chunk_scan(q, k_f, v, logf_f)
    flip = lambda t: jnp.flip(t, axis=1)
    o_b = flip(gla_chunk_scan(flip(q), flip(k_b), flip(v), flip(logf_b)))
    o = o_f + o_b
    o = o * lax.rsqrt(jnp.mean(o * o, axis=-1, keepdims=True) + NORM_EPS)
    o = o * norm_w.astype(jnp.float32).reshape(HG_HEADS, HG_DV)
    o = o.reshape(B, S, HG_WIDTH) * jax.nn.silu(g_raw.astype(jnp.float32))
    return o.astype(dt)


def dilated_window_attention(q, k, v, dilation, radius):
    B, S, H, hd = q.shape
    L = S // dilation
    blk = radius
    nb = -(-L // blk)
    Lp = nb * blk
    Bd = B * dilation

    def residues(t):
        return t.reshape(B, L, dilation, H, hd).transpose(0, 2, 1, 3, 4).reshape(Bd, L, H, hd)

    qs = jnp.pad(residues(q), ((0, 0), (0, Lp - L), (0, 0), (0, 0))).reshape(Bd, nb, blk, H, hd)

    def key_windows(t):
        tp = jnp.pad(residues(t), ((0, 0), (blk, Lp - L + blk), (0, 0), (0, 0))).reshape(Bd, nb + 2, blk, H, hd)
        return jnp.concatenate([tp[:, :-2], tp[:, 1:-1], tp[:, 2:]], axis=2)

    kw = key_windows(k)
    vw = key_windows(v)
    qpos = jnp.arange(nb)[:, None] * blk + jnp.arange(blk)[None, :]
    kpos = jnp.arange(nb)[:, None] * blk - blk + jnp.arange(3 * blk)[None, :]
    valid = ((jnp.abs(qpos[:, :, None] - kpos[:, None, :]) <= radius)
             & (kpos >= 0)[:, None, :] & (kpos < L)[:, None, :])
    s = jnp.einsum('bnqhd,bnkhd->bnhqk', qs, kw)
    s = jnp.where(valid[None, :, None], s, -jnp.inf)
    m = jnp.max(s, axis=-1, keepdims=True)
    p = jnp.exp(s - m)
    l = jnp.sum(p, axis=-1, keepdims=True)
    o = jnp.einsum('bnhqk,bnkhd->bnqhd', p, vw) / jnp.transpose(l, (0, 1, 3, 2, 4))
    lse = jnp.transpose((m + jnp.log(l))[..., 0], (0, 1, 3, 2))

    def back(t):
        t = t.reshape(Bd, Lp, *t.shape[3:])[:, :L]
        t = t.reshape(B, dilation, L, *t.shape[2:])
        return jnp.swapaxes(t, 1, 2).reshape(B, S, *t.shape[3:])

    return back(o), back(lse)


def dilated_attention_mixer(q_raw, k_raw, v_raw):
    B, S, _ = q_raw.shape
    dt = q_raw.dtype
    pos = jnp.arange(S)

    def heads(t):
        return t.astype(jnp.float32).reshape(B, S, ATTN_HEADS, HEAD_DIM)

    q = partial_rotary(heads(q_raw), pos) * (HEAD_DIM ** -0.5)
    k = partial_rotary(heads(k_raw), pos)
    v = heads(v_raw)
    outs, lses = [], []
    for g in range(ATTN_GROUPS):
        sl = slice(g * ATTN_HEADS_PER_GROUP, (g + 1) * ATTN_HEADS_PER_GROUP)
        radius = ATTN_WINDOWS[g] // (2 * ATTN_DILATIONS[g])
        o_g, lse_g = dilated_window_attention(q[:, :, sl], k[:, :, sl], v[:, :, sl],
                                              ATTN_DILATIONS[g], radius)
        outs.append(o_g)
        lses.append(lse_g)
    alpha = jax.nn.softmax(jnp.stack(lses, axis=0), axis=0)
    o = jnp.stack(outs, axis=0) * alpha[..., None]
    return jnp.moveaxis(o, 0, 2).reshape(B, S, ATTN_WIDTH).astype(dt)


def expert_choice_moe(x, w_router, w_gate, w_up, w_down):
    B, S, D = x.shape
    n_tok = B * S
    capacity = EC_CAPACITY * n_tok // N_EXPERTS
    xf = x.reshape(n_tok, D)
    aff = jax.nn.softmax((xf @ w_router).astype(jnp.float32), axis=-1)
    gate, idx = lax.top_k(aff.T, capacity)
    xe = xf[idx]
    h = (jax.nn.silu(jnp.einsum('ecd,edf->ecf', xe, w_gate))
         * jnp.einsum('ecd,edf->ecf', xe, w_up))
    ye = jnp.einsum('ecf,efd->ecd', h, w_down) * gate[..., None].astype(x.dtype)
    y = jnp.zeros((n_tok, D), x.dtype).at[idx.reshape(-1)].add(ye.reshape(-1, D))
    return y.reshape(B, S, D)


def trunk(x, norm1_w, w_in, lb_fwd_logits, lb_bwd_logits, hgrn_norm_w, w_out,
          norm2_w, w_router, w_gate, w_up, w_down, final_norm_w):
    lb_f_all = layer_lower_bounds(lb_fwd_logits)
    lb_b_all = layer_lower_bounds(lb_bwd_logits)
    splits = [HG_WIDTH * j for j in range(1, 6)] + [5 * HG_WIDTH + ATTN_WIDTH, 5 * HG_WIDTH + 2 * ATTN_WIDTH]
    for l in range(DEPTH):
        h = rms_norm(x, norm1_w[l])
        proj = h @ w_in[l]
        q_h, zf, zb, i_h, g_h, q_a, k_a, v_a = jnp.split(proj, splits, axis=-1)
        hg_out = hgrn2_mixer(q_h, zf, zb, i_h, g_h, lb_f_all[l], lb_b_all[l], hgrn_norm_w[l])
        at_out = dilated_attention_mixer(q_a, k_a, v_a)
        x = x + jnp.concatenate([hg_out, at_out], axis=-1) @ w_out[l]
        x = x + expert_choice_moe(rms_norm(x, norm2_w[l]), w_router[l], w_gate[l], w_up[l], w_down[l])
    return rms_norm(x, final_norm_w)


def setup_inputs(seed: int = 0) -> dict:
    key = jax.random.key(seed)
    ks = jax.random.split(key, 14)
    f32 = jnp.float32
    nrm = lambda k, shape: jax.random.normal(k, shape, f32)
    return {
        "x_prompt": nrm(ks[0], (BATCH, SEQ, D_MODEL)),
        "x_sample": nrm(ks[1], (DEC_BATCH, DEC_SEQ, D_MODEL)),
        "norm1_w": 1.0 + 0.02 * nrm(ks[2], (DEPTH, D_MODEL)),
        "w_in": nrm(ks[3], (DEPTH, D_MODEL, IN_COLS)) * D_MODEL ** -0.5,
        "lb_fwd_logits": 0.1 * nrm(ks[4], (DEPTH, HG_WIDTH)),
        "lb_bwd_logits": 0.1 * nrm(ks[5], (DEPTH, HG_WIDTH)),
        "hgrn_norm_w": 1.0 + 0.02 * nrm(ks[6], (DEPTH, HG_WIDTH)),
        "w_out": nrm(ks[7], (DEPTH, MIX_WIDTH, D_MODEL)) * MIX_WIDTH ** -0.5,
        "norm2_w": 1.0 + 0.02 * nrm(ks[8], (DEPTH, D_MODEL)),
        "w_router": nrm(ks[9], (DEPTH, D_MODEL, N_EXPERTS)) * D_MODEL ** -0.5,
        "w_gate": nrm(ks[10], (DEPTH, N_EXPERTS, D_MODEL, EXPERT_FF)) * D_MODEL ** -0.5,
        "w_up": nrm(ks[11], (DEPTH, N_EXPERTS, D_MODEL, EXPERT_FF)) * D_MODEL ** -0.5,
        "w_down": nrm(ks[12], (DEPTH, N_EXPERTS, EXPERT_FF, D_MODEL)) * EXPERT_FF ** -0.5,
        "final_norm_w": 1.0 + 0.02 * nrm(ks[13], (D_MODEL,)),
    }


def reference(x_prompt, x_sample, norm1_w, w_in, lb_fwd_logits, lb_bwd_logits, hgrn_norm_w,
              w_out, norm2_w, w_router, w_gate, w_up, w_down, final_norm_w):
    y_prompt = trunk(x_prompt, norm1_w, w_in, lb_fwd_logits, lb_bwd_logits, hgrn_norm_w, w_out,
                     norm2_w, w_router, w_gate, w_up, w_down, final_norm_w)
    y_sample = trunk(x_sample, norm1_w, w_in, lb_fwd_logits, lb_bwd_logits, hgrn_norm_w, w_out,
                     norm2_w, w_router, w_gate, w_up, w_down, final_norm_w)
    return (y_prompt, y_sample)
```

```python
import math
from contextlib import ExitStack

import numpy as np
import ml_dtypes

import concourse.bass as bass
import concourse.mybir as mybir
from concourse.bass_utils import run_bass_kernel_spmd

F32 = mybir.dt.float32
BF16 = mybir.dt.bfloat16
I32 = mybir.dt.int32
AF = mybir.ActivationFunctionType
ALU = mybir.AluOpType
AX = mybir.AxisListType

NCORES = 8
D = 1024
DEPTH = 4
NTOK = 8192
TS = 512
NT = NTOK // TS
HG_H = 5
CH = 64
NCHUNK = NTOK // CH
NE = 16
EPS = 1e-6
ROPE_THETA = 500000.0
WA = 3200
WB = 1920
SCALE_Q = 0.125
MASKNEG = -30000.0

ENGS = ("pe", "act", "dve", "pool", "sp")


class Op:
    __slots__ = ("eng", "fn", "r", "w", "dma", "key", "idx", "sig", "n", "bar", "inc", "ep")

    def __init__(self, eng, fn, r, w, dma, key):
        self.eng = eng
        self.fn = fn
        self.r = r
        self.w = w
        self.dma = dma
        self.key = key
        self.sig = False
        self.n = 0
        self.bar = False
        self.inc = 16


class Prog:
    def __init__(self, nc):
        self.nc = nc
        self.ops = []
        self.epoch = 0

    def new_epoch(self):
        self.epoch += 1

    def add(self, eng, fn, r=(), w=(), dma=False, key=None):
        op = Op(eng, fn, tuple(r), tuple(w), dma, key)
        op.ep = self.epoch
        op.idx = len(self.ops)
        self.ops.append(op)
        return op

    def pe(self, fn, r=(), w=()):
        return self.add("pe", fn, r, w)

    def act(self, fn, r=(), w=()):
        return self.add("act", fn, r, w)

    def dve(self, fn, r=(), w=()):
        return self.add("dve", fn, r, w)

    def pool(self, fn, r=(), w=()):
        return self.add("pool", fn, r, w)

    def dma(self, eng, fn, r=(), w=(), key=None, inc=16):
        assert key is not None
        op = self.add(eng, fn, r, w, dma=True, key=key)
        op.inc = inc
        return op

    def barrier(self):
        for e in ENGS:
            op = self.add(e, None)
            op.bar = True

    def finalize(self, stack):
        nc = self.nc
        ops = self.ops
        last_w = {}
        readers = {}
        deps = [None] * len(ops)
        last_on = {e: None for e in ENGS}
        bar_waits = {}
        for op in ops:
            if op.bar:
                bar_waits[op.idx] = dict(last_on)
                continue
            d = set()
            for b in op.r:
                j = last_w.get(b)
                if j is not None:
                    d.add(j)
            for b in op.w:
                j = last_w.get(b)
                if j is not None:
                    d.add(j)
                rs = readers.get(b)
                if rs:
                    d.update(rs.values())
            for b in op.r:
                rk = ("dma", op.idx) if op.dma else op.eng
                readers.setdefault(b, {})[rk] = op.idx
            for b in op.w:
                last_w[b] = op.idx
                readers[b] = {}
            d.discard(op.idx)
            deps[op.idx] = d
            if not op.dma:
                last_on[op.eng] = op.idx
        for op in ops:
            if op.bar:
                for e, j in bar_waits[op.idx].items():
                    if j is not None and e != op.eng:
                        ops[j].sig = True
                continue
            keep = []
            for j in deps[op.idx]:
                pj = ops[j]
                if (not pj.dma) and (not op.dma) and pj.eng == op.eng:
                    if op.eng == "pe":
                        continue
                    raw = any(b in pj.w for b in op.r) or any(b in pj.w for b in op.w)
                    if not raw:
                        continue
                keep.append(j)
                if not pj.dma:
                    pj.sig = True
            deps[op.idx] = keep
        cnt = {}
        keycnt = {}
        for op in ops:
            if op.bar:
                continue
            if op.dma:
                keycnt[op.key] = keycnt.get(op.key, 0) + 1
                op.n = keycnt[op.key]
            elif op.sig:
                ck = (op.eng, op.ep)
                cnt[ck] = cnt.get(ck, 0) + 1
                op.n = cnt[ck]
        keycnt2 = {}
        waits_for = [None] * len(ops)
        waited = {e: {} for e in ENGS}
        for op in ops:
            w = {}
            if op.bar:
                for e, j in bar_waits[op.idx].items():
                    if j is not None and e != op.eng:
                        w[("e", e, ops[j].ep)] = ops[j].n
                for k, c in keycnt2.items():
                    w[("k", k)] = c
            else:
                for j in deps[op.idx]:
                    pj = ops[j]
                    if pj.dma:
                        sem = ("k", pj.key)
                        val = keycnt2.get(pj.key, 0)
                    else:
                        sem = ("e", pj.eng, pj.ep)
                        val = pj.n
                    if val > w.get(sem, 0):
                        w[sem] = val
            wl = []
            for sem, val in w.items():
                if val <= 0 or waited[op.eng].get(sem, 0) >= val:
                    continue
                waited[op.eng][sem] = val
                wl.append((sem, val))
            waits_for[op.idx] = wl
            if op.dma:
                keycnt2[op.key] = keycnt2.get(op.key, 0) + op.inc
        sems = {}
        for (e, ep), c in cnt.items():
            sems[("e", e, ep)] = stack.enter_context(nc.semaphore(f"c_{e}{ep}"))
        for k in keycnt2:
            sems[("k", k)] = stack.enter_context(nc.semaphore("k_" + str(k)))
        self.nsems = len(sems)
        self.sig_counts = cnt
        block = stack.enter_context(nc.Block())
        per = {e: [op for op in ops if op.eng == e] for e in ENGS}

        def emit(eng_name, engine):
            for op in per[eng_name]:
                for sem, val in waits_for[op.idx]:
                    engine.wait_ge(sems[sem], val)
                if op.bar:
                    continue
                ins = op.fn(engine)
                if op.dma:
                    ins.then_inc(sems[("k", op.key)], op.inc)
                elif op.sig:
                    ins.then_inc(sems[("e", op.eng, op.ep)], 1)
            if eng_name == "sp":
                for k, c in keycnt2.items():
                    engine.wait_ge(sems[("k", k)], c)
                for (e, ep), c in cnt.items():
                    engine.wait_ge(sems[("e", e, ep)], c)

        @block.tensor
        def _(e):
            emit("pe", e)

        @block.scalar
        def _(e):
            emit("act", e)

        @block.vector
        def _(e):
            emit("dve", e)

        @block.gpsimd
        def _(e):
            emit("pool", e)

        @block.sync
        def _(e):
            emit("sp", e)


class Arena:
    def __init__(self, nc, lo, hi, tag):
        self.nc, self.lo, self.hi, self.cur, self.n, self.tag = nc, lo, hi, lo, 0, tag

    def reset(self):
        self.cur = self.lo

    def alloc(self, name, shape, dtype):
        esz = 4 if dtype in (F32, I32) else 2
        size = esz
        for s in shape[1:]:
            size *= s
        size = (size + 31) // 32 * 32
        off = self.cur
        self.cur += size
        assert self.cur <= self.hi, (name, self.cur, self.hi)
        self.n += 1
        return self.nc.alloc_sbuf_tensor_at(f"{self.tag}{self.n}_{name}", list(shape), dtype, offset=off)


class Builder:
    def __init__(self, nlayers=DEPTH, debug=None, stop=None, nt=NT, nexp=NE):
        self.nlayers = nlayers
        self.nt = nt
        self.nexp = nexp
        self.debug = debug or ()
        self.stop = stop
        self.nc = bass.Bass("TRN2", target_bir_lowering=False)
        self.P = Prog(self.nc)
        self.pers = Arena(self.nc, 18 * 1024, 54 * 1024, "p")
        self.ph = Arena(self.nc, 54 * 1024, 223 * 1024, "t")
        self.uid = 0

    def din(self, name, shape, dtype=F32):
        return self.nc.dram_tensor(name, list(shape), dtype, kind="ExternalInput").ap()

    def dscr(self, name, shape, dtype):
        kind = "ExternalOutput" if name in self.debug else "Internal"
        return self.nc.dram_tensor(name, list(shape), dtype, kind=kind).ap()

    def build(self):
        nc, P = self.nc, self.P
        with ExitStack() as st:
            self.st = st
            self.declare()
            self.setup_consts()
            for l in range(self.nlayers):
                self.layer(l)
                if self.stop is not None and l == self.nlayers - 1:
                    break
            P.barrier()
            P.finalize(st)
        return nc

    def declare(self):
        nc = self.nc
        self.x_in = self.din("x", [NTOK, D])
        self.y_out = nc.dram_tensor("y", [NTOK, D], F32, kind="ExternalOutput").ap()
        self.w_in = self.din("w_in", [DEPTH, D, WA + WB])
        self.w_out = self.din("w_out", [DEPTH, D, D])
        self.w_router = self.din("w_router", [DEPTH, D, NE])
        ne_decl = NE if self.stop is None else 1
        self.w_gate = self.din("w_gate", [DEPTH, ne_decl, D, D])
        self.w_up = self.din("w_up", [DEPTH, ne_decl, D, D])
        self.w_down = self.din("w_down", [DEPTH, ne_decl, D, D])
        self.c_n1 = self.din("c_n1", [128, DEPTH, 8])
        self.c_n2 = self.din("c_n2", [128, DEPTH, 8])
        self.c_hgw = self.din("c_hgw", [128, DEPTH, HG_H])
        self.c_fnw = self.din("c_fnw", [1, D])
        self.c_lbl = self.din("c_lbl", [128, 2, HG_H, DEPTH])
        self.c_ropeC = self.din("c_ropeC", [128, NTOK])
        self.c_ropeS = self.din("c_ropeS", [128, NTOK])
        self.c_ident = self.din("c_ident", [128, 128])
        self.c_rst = self.din("c_rst", [128, TS])
        self.c_hmask = self.din("c_hmask", [CH, 2, CH])
        self.c_flags = self.din("c_flags", [128, 2, NCHUNK])
        self.c_amask = self.din("c_amask", [128, 3, 2, 128])
        self.XA = self.dscr("XA", [NTOK, D], F32)
        self.XM = self.dscr("XM", [NTOK, D], F32)
        self.HT = self.dscr("HT", [128, 8, NTOK], BF16)
        self.HG = self.dscr("HG", [128, HG_H, 6, NTOK], BF16)
        self.AT = self.dscr("AT", [128, 9, NTOK], BF16)
        self.OF = self.dscr("OF", [128, 2, HG_H, NTOK], BF16)
        self.AO = self.dscr("AO", [128, 3, NTOK], BF16)
        self.XN = self.dscr("XN", [128, 8, NTOK], BF16)
        if "GT" in self.debug:
            self.GTd = self.nc.dram_tensor("GTd", [128, NTOK // 128, NE], F32, kind="ExternalOutput").ap()
            self.THd = self.nc.dram_tensor("THd", [64, 16], F32, kind="ExternalOutput").ap()
        self.AFL = self.dscr("AFL", [NE, NTOK], F32)
        self.AFG = self.dscr("AFG", [4 * NE, NTOK], F32)
        self.c_gsum = self.din("c_gsum", [64, 64])
        self.c_dsel = self.din("c_dsel", [64, NE])

    def setup_consts(self):
        nc, P, A = self.nc, self.P, self.pers
        self.ident_bf = A.alloc("identb", [128, 128], BF16)
        self.ident_f = A.alloc("identf", [128, 128], F32)
        self.ones_bf = A.alloc("onesb", [128, 128], BF16)
        self.n1 = A.alloc("n1", [128, DEPTH, 8], F32)
        self.n2 = A.alloc("n2", [128, DEPTH, 8], F32)
        self.hgw = A.alloc("hgw", [128, DEPTH, HG_H], F32)
        self.lb = A.alloc("lb", [128, 2, HG_H, DEPTH], F32)
        self.oml = A.alloc("oml", [128, 2, HG_H, DEPTH], F32)
        self.rst = A.alloc("rst", [128, TS], F32)
        self.hmask = A.alloc("hmask", [CH, 2, CH], F32)
        self.flags = A.alloc("flags", [128, 2, NCHUNK], F32)
        self.scal = A.alloc("scal", [128, 2, HG_H, 3, NCHUNK], F32)
        self.aff = A.alloc("aff", [128, NTOK // 128, NE], F32)
        self.amask = A.alloc("amask", [128, 3, 2, 128], BF16)
        self.epsb = A.alloc("epsb", [128, 1], F32)
        P.dve(lambda e: e.memset(self.epsb[:], EPS), w=["epsb"])
        self.psF = [self.st.enter_context(nc.psum_tensor(f"psF{j}", [128, 512], F32)) for j in range(7)]
        psb = self.st.enter_context(nc.psum_tensor("psB", [128, 1024], BF16))
        self.psB = [psb, psb]
        self.psBt = psb
        lbt = A.alloc("lbt", [128, 2, HG_H, DEPTH], F32)
        lbs = A.alloc("lbs", [128, 2, HG_H, 1], F32)

        def ld(dst, src, name, eng="sp"):
            P.dma(eng, lambda e: e.dma_start(out=dst, in_=src), w=[name], key="c_" + name)

        ld(self.ident_f[:], self.c_ident[:, :], "identf")
        ld(self.ident_bf[:], self.c_ident[:, :], "identb", "pool")
        ld(self.n1[:], self.c_n1[:, :, :], "n1")
        ld(self.n2[:], self.c_n2[:, :, :], "n2")
        ld(self.hgw[:], self.c_hgw[:, :, :], "hgw")
        ld(lbt[:], self.c_lbl[:, :, :, :], "lbt")
        ld(self.rst[:], self.c_rst[:, :], "rst")
        ld(self.hmask[:], self.c_hmask[:, :, :], "hmask")
        ld(self.flags[:], self.c_flags[:, :, :], "flags")
        ld(self.amask[:], self.c_amask[:, :, :, :], "amask", "pool")
        P.dve(lambda e: e.memset(self.ones_bf[:], 1.0), w=["onesb"])
        P.act(lambda e: e.activation(out=lbt[:], in_=lbt[:], func=AF.Exp), r=["lbt"], w=["lbt"])
        P.dve(lambda e: e.tensor_reduce(out=lbs[:], in_=lbt[:], axis=AX.X, op=ALU.add), r=["lbt"], w=["lbs"])
        P.dve(lambda e: e.reciprocal(out=lbs[:], in_=lbs[:]), r=["lbs"], w=["lbs"])
        P.dve(lambda e: e.tensor_tensor(out=lbt[:], in0=lbt[:], in1=lbs[:].to_broadcast([128, 2, HG_H, DEPTH]),
                                        op=ALU.mult), r=["lbt", "lbs"], w=["lbt"])
        P.dve(lambda e: e.memset(self.lb[:, :, :, 0:1], 0.0), w=["lb"])
        for l in range(1, DEPTH):
            P.dve(lambda e, l=l: e.tensor_tensor(out=self.lb[:, :, :, l:l + 1], in0=self.lb[:, :, :, l - 1:l],
                                                 in1=lbt[:, :, :, l:l + 1], op=ALU.add),
                  r=["lbt", "lb"], w=["lb"])
        P.dve(lambda e: e.tensor_scalar(out=self.oml[:], in0=self.lb[:], scalar1=-1.0, scalar2=1.0,
                                        op0=ALU.mult, op1=ALU.add), r=["lb"], w=["oml"])
        for i in range(4):
            sl = slice(i * 2048, (i + 1) * 2048)
            P.dma("sp", lambda e, sl=sl: e.dma_start(out=self.XA[sl, :], in_=self.x_in[sl, :]),
                  w=[("XA", j) for j in range(i * 4, i * 4 + 4)], key="xcopy")


    def layer(self, l):
        self.phase_p1a(l)
        if self.stop == "p1a":
            return
        self.phase_p1b(l)
        if self.stop == "p1b":
            return
        self.phase_p2(l)
        if self.stop == "p2":
            return
        self.phase_p3(l)
        if self.stop == "p3":
            return
        self.phase_p4(l)
        if self.stop == "p4":
            return
        self.P.new_epoch()
        self.phase_p67(l, last=(l == self.nlayers - 1))
        self.P.new_epoch()

    def phase_p1a(self, l):
        nc, P, A = self.nc, self.P, self.ph
        P.barrier()
        A.reset()
        st = self.st
        win = A.alloc("win", [128, 8, WA], BF16)
        xt = A.alloc("xt", [128, 4, D], F32)
        xs = A.alloc("xs", [128, 4, D], BF16)
        junk = A.alloc("junk", [128, D], BF16)
        ss = A.alloc("ss", [128, 4], F32)
        rstd = A.alloc("rstd", [128, 4], F32)
        hT = [A.alloc(f"hT{j}", [128, 8, TS], BF16) for j in range(2)]
        OH = [A.alloc(f"OH{j}", [128, 6, TS], BF16) for j in range(2)]
        NSET = 2
        T = []
        for j in range(NSET):
            d = {}
            for nm in ("eq", "qsb", "eg", "gsb"):
                d[nm] = A.alloc(f"{nm}{j}", [128, TS], F32)
            for dr in range(2):
                for nm in ("t0", "t1", "b", "E1"):
                    d[(nm, dr)] = A.alloc(f"{nm}{j}{dr}", [128, TS], F32)
                d[("sm", dr)] = A.alloc(f"sm{j}{dr}", [128, 3, 8], F32)
            T.append(d)
        psF, psB = self.psF, self.psB

        for k in range(8):
            P.dma("pool", lambda e, k=k: e.dma_start(out=win[:, k, :], in_=self.w_in[l, k * 128:(k + 1) * 128, 0:WA]),
                  w=[("win", k)], key="win")
        n1b = self.n1[:, l, :]

        def norm_tile(i):
            tsl = slice(i * TS, (i + 1) * TS)
            P.dma("sp", lambda e: e.dma_start(
                out=xt[:], in_=self.XA[i * TS:(i + 1) * TS, :].rearrange("(a p) f -> p a f", p=128)),
                r=[("XA", i)], w=["xt"], key="xt")
            P.dve(lambda e: e.memset(ss[:], 0.0), w=["ss"])
            for a in range(4):
                P.act(lambda e, a=a: e.activation(out=junk[:], in_=xt[:, a, :], func=AF.Square,
                                                  accum_out=ss[:, a:a + 1]),
                      r=["xt", "ss"], w=["junk", "ss"])
            P.act(lambda e: e.activation(out=rstd[:], in_=ss[:], func=AF.Ln, scale=1.0 / D, bias=self.epsb[:]),
                  r=["ss", "epsb"], w=["rstd"])
            P.act(lambda e: e.activation(out=rstd[:], in_=rstd[:], func=AF.Exp, scale=-0.5), r=["rstd"], w=["rstd"])
            for a in range(4):
                P.dve(lambda e, a=a: e.tensor_scalar(out=xs[:, a, :], in0=xt[:, a, :], scalar1=rstd[:, a:a + 1],
                                                     scalar2=None, op0=ALU.mult),
                      r=["xt", "rstd"], w=[("xs", a)])
            h = hT[i % 2]
            hname = ("hT", i % 2)
            for a in range(4):
                pb = psB[0]
                pbn = ("psB", 0)
                for k in range(8):
                    P.pe(lambda e, a=a, k=k, pb=pb: e.transpose(pb[:, k * 128:(k + 1) * 128],
                                                                xs[:, a, k * 128:(k + 1) * 128], self.ident_bf[:]),
                         r=[("xs", a), "identb"], w=[pbn])
                P.dve(lambda e, a=a, pb=pb: e.tensor_tensor(
                    out=h[:, :, a * 128:(a + 1) * 128],
                    in0=pb[:].rearrange("p (k t) -> p k t", k=8),
                    in1=n1b.unsqueeze(2).to_broadcast([128, 8, 128]), op=ALU.mult),
                    r=[pbn, "n1"], w=[hname])
            P.dma("pool", lambda e: e.dma_start(out=self.HT[:, :, tsl], in_=h[:]),
                  r=[hname], w=[("HT", i)], key=f"hTst{i % 2}")

        def emit(chain):
            for eng, fn, r, w in chain:
                P.add(eng, fn, r, w)

        def zipchains(chains):
            n = max(len(c) for c in chains)
            for j in range(n):
                for c in chains:
                    if j < len(c):
                        P.add(*c[j])

        def dir_chain(i, hd, dr, bz, tset, tn, oh, ohn, qsb):
            t0, t1, b, E1, sm = (tset[(nm, dr)] for nm in ("t0", "t1", "b", "E1", "sm"))
            n0, n1_, nb, nE, ns = (tn((nm, dr)) for nm in ("t0", "t1", "b", "E1", "sm"))
            lbv = self.lb[:, dr, hd, l:l + 1]
            c = []
            c.append(("act", lambda e: e.activation(out=t0[:], in_=psF[bz][:], func=AF.Exp, scale=-1.0),
                      [("psF", bz)], [n0]))
            c.append(("act", lambda e: e.activation(out=t1[:], in_=t0[:], func=AF.Ln, scale=lbv, bias=1.0),
                      [n0, "lb"], [n1_]))
            c.append(("act", lambda e: e.activation(out=b[:], in_=t0[:], func=AF.Ln, bias=1.0), [n0], [nb]))
            c.append(("dve", lambda e: e.tensor_tensor(out=b[:], in0=t1[:], in1=b[:], op=ALU.subtract),
                      [n1_, nb], [nb]))
            c.append(("act", lambda e: e.activation(out=t0[:], in_=b[:], func=AF.Exp), [nb], [n0]))
            if dr == 0:
                c.append(("dve", lambda e: e.tensor_tensor_scan(
                    out=b[:], data0=self.rst[:], data1=b[:], initial=0.0, op0=ALU.mult, op1=ALU.add),
                    [nb, "rst"], [nb]))
                bv = b[:].rearrange("p (c j u) -> p c j u", c=8, j=2)[:, :, :, 31]
                mrow, Brow = 0, 1
            else:
                c.append(("dve", lambda e: e.tensor_tensor_scan(
                    out=b[:, ::-1], data0=self.rst[:], data1=b[:, ::-1], initial=0.0,
                    op0=ALU.mult, op1=ALU.add), [nb, "rst"], [nb]))
                bv = b[:].rearrange("p (c j u) -> p c j u", c=8, j=2)[:, :, :, 0]
                mrow, Brow = 1, 0
            c.append(("dve", lambda e: e.tensor_copy(out=sm[:, 0:2, :].rearrange("p j c -> p c j"), in_=bv),
                      [nb], [ns]))
            c.append(("dve", lambda e: e.tensor_tensor(out=sm[:, 2, :], in0=sm[:, Brow, :], in1=sm[:, mrow, :],
                                                       op=ALU.subtract), [ns], [ns]))
            for j, row in enumerate((Brow, mrow, 2)):
                c.append(("act", lambda e, row=row, j=j: e.activation(
                    out=self.scal[:, dr, hd, j, i * 8:(i + 1) * 8], in_=sm[:, row, :], func=AF.Exp),
                    [ns], [("scal", dr, hd, i)]))
            c.append(("dve", lambda e: e.tensor_tensor(
                out=b[:].rearrange("p (c u) -> p c u", c=8),
                in0=b[:].rearrange("p (c u) -> p c u", c=8),
                in1=sm[:, mrow, :].unsqueeze(2).to_broadcast([128, 8, CH]), op=ALU.subtract),
                [nb, ns], [nb]))
            c.append(("act", lambda e: e.activation(out=E1[:], in_=b[:], func=AF.Exp), [nb], [nE]))
            c.append(("act", lambda e: e.activation(out=b[:], in_=b[:], func=AF.Exp, scale=-1.0), [nb], [nb]))
            c.append(("dve", lambda e: e.tensor_tensor(out=oh[:, 2 * dr, :], in0=qsb[:], in1=E1[:], op=ALU.mult),
                      [tn("qsb"), nE], [ohn]))
            c.append(("dve", lambda e: e.scalar_tensor_tensor(
                out=oh[:, 2 * dr + 1, :], in0=t0[:], scalar=1.0, in1=b[:], op0=ALU.subtract, op1=ALU.mult),
                [n0, nb], [ohn]))
            return c

        def head_body(i, hd, g):
            tsl = slice(i * TS, (i + 1) * TS)
            h = hT[i % 2]
            hname = ("hT", i % 2)
            si = g % NSET
            tset = T[si]
            tn = lambda nm: (nm, si)
            oh = OH[g % 2]
            ohn = ("OH", g % 2)
            bzf, bzb = (0, 1) if g % 2 == 0 else (2, 3)
            bq, bi, bg = 4, 5, 6
            for c, bk in ((5 + hd, bzf), (10 + hd, bzb), (hd, bq), (15 + hd, bi), (20 + hd, bg)):
                for k in range(8):
                    P.pe(lambda e, bk=bk, k=k, c=c: e.matmul(psF[bk][:], win[:, k, c * 128:(c + 1) * 128],
                                                             h[:, k, :], start=(k == 0), stop=(k == 7)),
                         r=[hname, ("win", k)], w=[("psF", bk)])
            eq, qsb, eg, gsb = tset["eq"], tset["qsb"], tset["eg"], tset["gsb"]
            cB = []
            cB.append(("act", lambda e: e.activation(out=eq[:], in_=psF[bq][:], func=AF.Exp, scale=-1.0),
                       [("psF", bq)], [tn("eq")]))
            cB.append(("act", lambda e: e.copy(out=oh[:, 4, :], in_=psF[bi][:]), [("psF", bi)], [ohn]))
            cB.append(("act", lambda e: e.activation(out=eg[:], in_=psF[bg][:], func=AF.Exp, scale=-1.0),
                       [("psF", bg)], [tn("eg")]))
            for ee, nm in ((eq, "eq"), (eg, "eg")):
                cB.append(("act", lambda e, ee=ee: e.activation(out=ee[:], in_=ee[:], func=AF.Ln, bias=1.0),
                           [tn(nm)], [tn(nm)]))
                cB.append(("act", lambda e, ee=ee: e.activation(out=ee[:], in_=ee[:], func=AF.Exp, scale=-1.0),
                           [tn(nm)], [tn(nm)]))
            cB.append(("dve", lambda e: e.tensor_tensor(out=qsb[:], in0=psF[bq][:], in1=eq[:], op=ALU.mult),
                       [("psF", bq), tn("eq")], [tn("qsb")]))
            cB.append(("dve", lambda e: e.tensor_tensor(out=oh[:, 5, :], in0=psF[bg][:], in1=eg[:], op=ALU.mult),
                       [("psF", bg), tn("eg")], [ohn]))
            chains = [dir_chain(i, hd, 0, bzf, tset, tn, oh, ohn, qsb),
                      dir_chain(i, hd, 1, bzb, tset, tn, oh, ohn, qsb), cB]
            zipchains(chains)
            P.dma("pool", lambda e: e.dma_start(out=self.HG[:, hd, :, tsl], in_=oh[:]),
                  r=[ohn], w=[("HG", hd, i)], key=f"ohst{g % 2}")

        g = 0
        norm_tile(0)
        for i in range(self.nt):
            for hd in range(HG_H):
                if hd == 2 and i + 1 < self.nt:
                    norm_tile(i + 1)
                head_body(i, hd, g)
                g += 1


    def phase_p1b(self, l):
        nc, P, A = self.nc, self.P, self.ph
        P.barrier()
        A.reset()
        psF = self.psF
        wb = A.alloc("wb", [128, 8, WB], BF16)
        hT = [A.alloc(f"hTb{j}", [128, 8, TS], BF16) for j in range(2)]
        rC = [A.alloc(f"rC{j}", [128, TS], F32) for j in range(2)]
        rS = [A.alloc(f"rS{j}", [128, TS], F32) for j in range(2)]
        AOt = [A.alloc(f"AOt{j}", [128, 9, TS], BF16) for j in range(2)]
        t1 = [A.alloc(f"ta{j}", [128, TS], F32) for j in range(4)]
        t2 = [A.alloc(f"tb{j}", [128, TS], F32) for j in range(4)]
        for k in range(8):
            P.dma("pool", lambda e, k=k: e.dma_start(out=wb[:, k, :],
                                                     in_=self.w_in[l, k * 128:(k + 1) * 128, WA:WA + WB]),
                  w=[("wb", k)], key="wb")

        def load(i):
            j = i % 2
            tsl = slice(i * TS, (i + 1) * TS)
            P.dma("sp", lambda e: e.dma_start(out=hT[j][:], in_=self.HT[:, :, tsl]),
                  r=[("HT", i)], w=[("hTb", j)], key=f"hTb{j}")
            P.dma("sp", lambda e: e.dma_start(out=rC[j][:], in_=self.c_ropeC[:, tsl]), w=[("rC", j)], key=f"rC{j}")
            P.dma("sp", lambda e: e.dma_start(out=rS[j][:], in_=self.c_ropeS[:, tsl]), w=[("rS", j)], key=f"rS{j}")

        def mm(bank, col, h, hn):
            for k in range(8):
                P.pe(lambda e, k=k: e.matmul(psF[bank][:], wb[:, k, col * 128:(col + 1) * 128], h[:, k, :],
                                             start=(k == 0), stop=(k == 7)),
                     r=[hn, ("wb", k)], w=[("psF", bank)])

        def body(i, u):
            j = i % 2
            tsl = slice(i * TS, (i + 1) * TS)
            h, hn = hT[j], ("hTb", j)
            ao, aon = AOt[j], ("AOt", j)
            for g in range(3):
                bv = 6
                mm(bv, 6 + g, h, hn)
                P.act(lambda e, g=g: e.copy(out=ao[:, 6 + g, :], in_=psF[bv][:]), r=[("psF", bv)], w=[aon])
                for which, (c0, c1, slot) in enumerate(((g, 9 + g, g), (3 + g, 12 + g, 3 + g))):
                    b0, b1 = (0, 1) if (u[0] % 2 == 0) else (2, 3)
                    ti = u[0] % 4
                    u[0] += 1
                    mm(b0, c0, h, hn)
                    mm(b1, c1, h, hn)
                    ta, tb = t1[ti], t2[ti]
                    P.dve(lambda e, ta=ta, b0=b0: e.tensor_tensor(out=ta[:], in0=psF[b0][:], in1=rC[j][:], op=ALU.mult),
                          r=[("psF", b0), ("rC", j)], w=[("ta", ti)])
                    P.dve(lambda e, tb=tb, b1=b1: e.tensor_tensor(out=tb[:], in0=psF[b1][:], in1=rS[j][:], op=ALU.mult),
                          r=[("psF", b1), ("rS", j)], w=[("tb", ti)])
                    P.pool(lambda e, ta=ta, tb=tb, slot=slot: e.tensor_tensor(out=ao[:, slot, :], in0=ta[:], in1=tb[:],
                                                                              op=ALU.add),
                           r=[("ta", ti), ("tb", ti)], w=[aon])
            P.dma("pool", lambda e: e.dma_start(out=self.AT[:, :, tsl], in_=ao[:]), r=[aon], w=[("AT", i)],
                  key=f"aost{j}")

        u = [0]
        load(0)
        for i in range(self.nt):
            if i + 1 < self.nt:
                load(i + 1)
            body(i, u)

    def phase_p2(self, l):
        nc, P, A = self.nc, self.P, self.ph
        P.barrier()
        A.reset()
        psF = self.psF
        NTl = self.nt
        HGt = [[A.alloc(f"HGt{d}{j}", [128, HG_H, 3, TS], BF16) for j in range(2)] for d in range(2)]
        Ost = [[A.alloc(f"Ost{d}{j}", [128, HG_H, TS], BF16) for j in range(2)] for d in range(2)]
        S = [A.alloc(f"S{d}", [128, HG_H, 128], F32) for d in range(2)]
        Sbf = [A.alloc(f"Sbf{d}", [128, HG_H, 128], BF16) for d in range(2)]
        tmp = [A.alloc(f"tmpU{d}", [128, HG_H, 128], F32) for d in range(2)]
        ktok = [A.alloc(f"ktok{d}", [CH, HG_H, 128], BF16) for d in range(2)]
        vtok = [A.alloc(f"vtok{d}", [CH, HG_H, 128], BF16) for d in range(2)]
        Am = [A.alloc(f"Am{d}", [CH, HG_H, CH], BF16) for d in range(2)]
        for d in range(2):
            for row in range(2):
                P.dve(lambda e, d=d, row=row: e.tensor_tensor(
                    out=self.scal[:, d, :, row, 0:NTl * 8], in0=self.scal[:, d, :, row, 0:NTl * 8],
                    in1=self.flags[:, d, 0:NTl * 8].unsqueeze(1).to_broadcast([128, HG_H, NTl * 8]), op=ALU.mult),
                    r=[("scal", d, hd, i) for hd in range(HG_H) for i in range(NTl)] + ["flags"],
                    w=[("scalf", d)])
            P.dve(lambda e, d=d: e.memset(S[d][:], 0.0), w=[("S", d)])
            P.dve(lambda e, d=d: e.memset(Sbf[d][:], 0.0), w=[("Sbf", d)])

        def load(d, i, slot):
            tsl = slice(i * TS, (i + 1) * TS)
            t = HGt[d][slot]
            for jj, cidx in enumerate((2 * d, 2 * d + 1, 4)):
                P.dma("sp", lambda e, jj=jj, cidx=cidx: e.dma_start(out=t[:, :, jj, :], in_=self.HG[:, :, cidx, tsl]),
                      r=[("HG", hd, i) for hd in range(HG_H)], w=[("HGt", d, slot)], key=f"hg{d}{slot}")

        def views(d):
            T1 = psF[0 + d][0:CH, :].bitcast(BF16)[:, 0:640].rearrange("p (h x) -> p h x", h=HG_H)
            T2b = (psF[6] if d == 0 else None)
            return T1

        psT1 = [psF[0][0:CH, :].bitcast(BF16), psF[1][0:CH, :].bitcast(BF16)]
        psT2 = [self.psBt[0:CH, 0:512], self.psBt[0:CH, 512:1024]]
        seq = []
        for step in range(NTl * 8):
            for d in range(2):
                seq.append((step, d))
        for d in range(2):
            load(d, 0 if d == 0 else NTl - 1, 0)
        def step_body(step, d):
            ti = step // 8
            cc_ = step % 8
            i = ti if d == 0 else NTl - 1 - ti
            cc = cc_ if d == 0 else 7 - cc_
            c = i * 8 + cc
            slot = ti % 2
            if cc_ == 0 and ti + 1 < NTl:
                load(d, (ti + 1) if d == 0 else NTl - 2 - ti, (ti + 1) % 2)
            t = HGt[d][slot]
            tn_ = ("HGt", d, slot)
            cols = slice(cc * CH, (cc + 1) * CH)
            ost, ostn = Ost[d][slot], ("Ost", d, slot)
            bT = 0 + d
            bA = 2 + d
            bO = 4 + d
            kT = psF[bT][0:CH, :].bitcast(BF16)
            vT = psF[6][0:CH, :].bitcast(BF16) if d == 0 else self.psBt[0:CH, :]
            vTn = ("psF", 6) if d == 0 else ("psB", 0)
            for hd in range(HG_H):
                P.pe(lambda e, hd=hd, kT=kT: e.transpose(kT[:, hd * 128:(hd + 1) * 128], t[:, hd, 1, cols],
                                                         self.ident_bf[:]),
                     r=[tn_, "identb"], w=[("psF", bT)])
            for hd in range(HG_H):
                P.pe(lambda e, hd=hd, vT=vT: e.transpose(vT[:, hd * 128:(hd + 1) * 128], t[:, hd, 2, cols],
                                                         self.ident_bf[:]),
                     r=[tn_, "identb"], w=[vTn])
            P.act(lambda e, kT=kT, d=d: e.copy(out=ktok[d][:].rearrange("p h x -> p (h x)"), in_=kT[:, 0:640]),
                  r=[("psF", bT)], w=[("ktok", d)])
            P.act(lambda e, vT=vT, d=d: e.copy(out=vtok[d][:].rearrange("p h x -> p (h x)"), in_=vT[:, 0:640]),
                  r=[vTn], w=[("vtok", d)])
            for hd in range(HG_H):
                P.pe(lambda e, hd=hd: e.matmul(psF[bA][0:CH, hd * CH:(hd + 1) * CH], t[:, hd, 1, cols],
                                               t[:, hd, 0, cols], start=True, stop=True),
                     r=[tn_], w=[("psF", bA)])
            P.dve(lambda e, d=d: e.tensor_tensor(
                out=Am[d][:], in0=psF[bA][0:CH, 0:HG_H * CH].rearrange("p (h x) -> p h x", h=HG_H),
                in1=self.hmask[:, d, :].unsqueeze(1).to_broadcast([CH, HG_H, CH]), op=ALU.mult),
                r=[("psF", bA), "hmask"], w=[("Am", d)])
            for hd in range(HG_H):
                P.pe(lambda e, hd=hd, d=d: e.matmul(psF[bO][:, hd * CH:(hd + 1) * CH], vtok[d][:, hd, :],
                                                    Am[d][:, hd, :], start=True, stop=False),
                     r=[("vtok", d), ("Am", d)], w=[("psF", bO)])
                P.pe(lambda e, hd=hd, d=d: e.matmul(psF[bO][:, hd * CH:(hd + 1) * CH], Sbf[d][:, hd, :],
                                                    t[:, hd, 0, cols], start=False, stop=True),
                     r=[("Sbf", d), tn_], w=[("psF", bO)])
            P.act(lambda e: e.copy(out=ost[:, :, cols],
                                   in_=psF[bO][:, 0:HG_H * CH].rearrange("p (h x) -> p h x", h=HG_H)),
                  r=[("psF", bO)], w=[ostn])
            for hd in range(HG_H):
                bank, off = (bT, hd * 128) if hd < 4 else (bA, 0)
                P.pe(lambda e, hd=hd, d=d, bank=bank, off=off: e.matmul(
                    psF[bank][:, off:off + 128], ktok[d][:, hd, :], vtok[d][:, hd, :], start=True, stop=True),
                    r=[("ktok", d), ("vtok", d)], w=[("psF", bank)])
            c1b = self.scal[:, d, :, 2, c:c + 1]
            decb = self.scal[:, d, :, 0, c:c + 1]
            P.dve(lambda e, d=d, c1b=c1b: e.tensor_tensor(
                out=tmp[d][:, 0:4, :], in0=psF[bT][:].rearrange("p (h x) -> p h x", h=4),
                in1=c1b[:, 0:4, :].to_broadcast([128, 4, 128]), op=ALU.mult),
                r=[("psF", bT), ("scal", d, 0, i), ("scal", d, 1, i), ("scal", d, 2, i), ("scal", d, 3, i), ("scalf", d)],
                w=[("tmpU", d)])
            P.dve(lambda e, d=d, c1b=c1b: e.tensor_tensor(
                out=tmp[d][:, 4:5, :], in0=psF[bA][:, 0:128].unsqueeze(1),
                in1=c1b[:, 4:5, :].to_broadcast([128, 1, 128]), op=ALU.mult),
                r=[("psF", bA), ("scal", d, 4, i), ("scalf", d)], w=[("tmpU", d)])
            P.dve(lambda e, d=d, decb=decb: e.tensor_tensor(
                out=S[d][:], in0=S[d][:], in1=decb.to_broadcast([128, HG_H, 128]), op=ALU.mult),
                r=[("S", d), ("scalf", d)] + [("scal", d, hd, i) for hd in range(HG_H)], w=[("S", d)])
            P.dve(lambda e, d=d: e.tensor_tensor(out=S[d][:], in0=S[d][:], in1=tmp[d][:], op=ALU.subtract),
                  r=[("S", d), ("tmpU", d)], w=[("S", d)])
            cn = c + 1 if d == 0 else c - 1
            if 0 <= cn < NTl * 8:
                emb = self.scal[:, d, :, 1, cn:cn + 1]
                inext = cn // 8
                P.pool(lambda e, d=d, emb=emb: e.tensor_tensor(
                    out=Sbf[d][:], in0=S[d][:], in1=emb.to_broadcast([128, HG_H, 128]), op=ALU.mult),
                    r=[("S", d), ("scalf", d)] + [("scal", d, hd, inext) for hd in range(HG_H)], w=[("Sbf", d)])
            if cc_ == 7:
                tsl = slice(i * TS, (i + 1) * TS)
                P.dma("pool", lambda e, ost=ost, d=d, tsl=tsl: e.dma_start(out=self.OF[:, d, :, tsl], in_=ost[:]),
                      r=[ostn], w=[("OF", d, i)], key=f"ofst{d}{slot}")

        for step, d in seq:
            step_body(step, d)

    def phase_p3(self, l):
        nc, P, A = self.nc, self.P, self.ph
        P.barrier()
        A.reset()
        psF = self.psF
        NS = max(1, self.nt // 4)
        SP = 2048
        qs = A.alloc("qs", [128, 3, SP], BF16)
        kv = A.alloc("kv", [128, 6, 2 * SP], BF16)
        UL = A.alloc("UL", [128, 2, 3, SP], F32)
        Pt = [A.alloc(f"Pt{j}", [128, 512], BF16) for j in range(2)]
        vpad = [A.alloc(f"vpad{j}", [128, 2, 128], BF16) for j in range(3)]
        onesp = A.alloc("onesp", [128, 2, 128], BF16)
        AOo = A.alloc("AOo", [128, 3, SP], BF16)
        Rc = A.alloc("Rc", [128, SP], F32)
        for j in range(3):
            P.dve(lambda e, j=j: e.memset(vpad[j][:], 0.0), w=[("vpad", j)])
        P.dve(lambda e: e.memset(onesp[:], 0.0), w=["onesp"])
        P.dve(lambda e: e.memset(onesp[:, 0, 0:64], 1.0), w=["onesp"])
        P.dve(lambda e: e.memset(onesp[:, 1, 64:128], 1.0), w=["onesp"])
        ntok = self.nt * TS
        cnt = {"s": 0, "v": 0}

        def span(s):
            t0 = s * SP
            lo, hi = max(0, t0 - 1024), min(ntok, t0 + SP + 1024)
            off = lo - (t0 - 1024)
            tiles_q = [("AT", i) for i in range(t0 // TS, (t0 + SP) // TS)]
            tiles_kv = [("AT", i) for i in range(lo // TS, hi // TS)]
            P.dma("sp", lambda e: e.dma_start(out=qs[:], in_=self.AT[:, 0:3, t0:t0 + SP]), r=tiles_q, w=["qs"], key="qsld")
            if off > 0:
                P.dve(lambda e: e.memset(kv[:, :, 0:off], 0.0), w=["kv"])
            if off + (hi - lo) < 2 * SP:
                P.dve(lambda e: e.memset(kv[:, :, off + (hi - lo):2 * SP], 0.0), w=["kv"])
            P.dma("sp", lambda e: e.dma_start(out=kv[:, :, off:off + (hi - lo)], in_=self.AT[:, 3:9, lo:hi]),
                  r=tiles_kv, w=["kv"], key="kvld")
            def kcols(r, rho, j):
                st_ = 1024 + (128 * j - 64) * r + rho
                return slice(st_, st_ + 127 * r + 1, r)

            def mk_v(g, r, rho, j):
                vi = cnt["v"] % 3
                cnt["v"] += 1
                reg = slice((vi % 4) * 128, (vi % 4) * 128 + 128)
                kc = kcols(r, rho, j)
                P.pe(lambda e: e.transpose(self.psBt[:, reg], kv[:, 3 + g, kc], self.ident_bf[:]),
                     r=["kv", "identb"], w=[("psB", 0)])
                P.act(lambda e: e.copy(
                    out=vpad[vi][:].rearrange("p h x -> p (h x)").rearrange("p (a b) -> p a b", b=64)[:, 0:4:3, :],
                    in_=self.psBt[:, reg].rearrange("p (a b) -> p a b", b=64)),
                    r=[("psB", 0)], w=[("vpad", vi)])
                return vi

            def block(g, r, rho, bi, vt, kinds):
                si = cnt["s"] % 2
                cnt["s"] += 1
                bS, bO = si, 2 + si
                qc = slice(128 * bi * r + rho, 128 * bi * r + rho + 127 * r + 1, r)
                for hh in range(2):
                    for tt in range(2):
                        c0 = (hh * 2 + tt) * 128
                        kc = kcols(r, rho, bi + tt)
                        P.pe(lambda e, hh=hh, c0=c0, kc=kc: e.matmul(
                            psF[bS][:, c0:c0 + 128], kv[hh * 64:(hh + 1) * 64, g, kc],
                            qs[hh * 64:(hh + 1) * 64, g, qc], start=True, stop=False),
                            r=["kv", "qs"], w=[("psF", bS)])
                        P.pe(lambda e, c0=c0, tt=tt, kd=kinds[tt]: e.matmul(
                            psF[bS][:, c0:c0 + 128], self.ident_bf[:], self.amask[:, kd, tt, :],
                            start=False, stop=True),
                            r=["identb", "amask"], w=[("psF", bS)])
                pt = Pt[si]
                P.act(lambda e: e.activation(out=pt[:], in_=psF[bS][:], func=AF.Exp, scale=SCALE_Q),
                      r=[("psF", bS)], w=[("Pt", si)])
                n = 0
                for hh in range(2):
                    for tt in range(2):
                        c0 = (hh * 2 + tt) * 128
                        P.pe(lambda e, hh=hh, tt=tt, c0=c0, n=n: e.matmul(
                            psF[bO][:, 0:128], vpad[vt[tt]][:, hh, :], pt[:, c0:c0 + 128],
                            start=(n == 0), stop=(n == 3)),
                            r=[("vpad", vt[tt]), ("Pt", si)], w=[("psF", bO)])
                        n += 1
                n = 0
                for hh in range(2):
                    for tt in range(2):
                        c0 = (hh * 2 + tt) * 128
                        P.pe(lambda e, hh=hh, c0=c0, n=n: e.matmul(
                            psF[bO][:, 128:256], onesp[:, hh, :], pt[:, c0:c0 + 128],
                            start=(n == 0), stop=(n == 3)),
                            r=["onesp", ("Pt", si)], w=[("psF", bO)])
                        n += 1
                P.act(lambda e: e.copy(out=UL[:, :, g, qc],
                                       in_=psF[bO][:, 0:256].rearrange("p (a b) -> p a b", a=2)),
                      r=[("psF", bO)], w=["UL"])

            for g, r in enumerate((1, 4, 16)):
                nb = (SP // r) // 128
                for rho in range(r):
                    vprev = mk_v(g, r, rho, 0)
                    for bi in range(nb):
                        vnext = mk_v(g, r, rho, bi + 1)
                        kinds = [0, 0]
                        if bi == 0:
                            kinds[0] = 1 if s == 0 else 2
                        if bi == nb - 1:
                            kinds[1] = 1 if s == NS - 1 else 2
                        block(g, r, rho, bi, (vprev, vnext), kinds)
                        vprev = vnext
            P.dve(lambda e: e.tensor_tensor(out=Rc[:], in0=UL[:, 1, 0, :], in1=UL[:, 1, 1, :], op=ALU.add),
                  r=["UL"], w=["Rc"])
            P.dve(lambda e: e.tensor_tensor(out=Rc[:], in0=Rc[:], in1=UL[:, 1, 2, :], op=ALU.add),
                  r=["UL", "Rc"], w=["Rc"])
            P.dve(lambda e: e.reciprocal(out=Rc[:], in_=Rc[:]), r=["Rc"], w=["Rc"])
            for g in range(3):
                P.dve(lambda e, g=g: e.tensor_tensor(out=AOo[:, g, :], in0=UL[:, 0, g, :], in1=Rc[:], op=ALU.mult),
                      r=["UL", "Rc"], w=["AOo"])
            P.dma("pool", lambda e: e.dma_start(out=self.AO[:, :, t0:t0 + SP], in_=AOo[:]), r=["AOo"],
                  w=[("AO", i) for i in range(t0 // TS, (t0 + SP) // TS)], key="aoo")

        for s in range(NS):
            span(s)

    def phase_p4(self, l):
        nc, P, A = self.nc, self.P, self.ph
        P.barrier()
        A.reset()
        psF = self.psF
        wo = A.alloc("wo", [128, 8, D], BF16)
        wr = A.alloc("wr", [128, 8, NE], BF16)
        OFt = [A.alloc(f"OFt{j}", [128, 2, HG_H, TS], BF16) for j in range(2)]
        sgt = [A.alloc(f"sgt{j}", [128, HG_H, TS], BF16) for j in range(2)]
        att = [A.alloc(f"att{j}", [128, 3, TS], BF16) for j in range(2)]
        xt = [A.alloc(f"xt4{j}", [128, 4, D], F32) for j in range(2)]
        ob = A.alloc("ob", [128, HG_H, TS], BF16)
        sq = A.alloc("sq", [128, HG_H, TS], BF16)
        lnt = [A.alloc(f"lnt{j}", [128, TS], F32) for j in range(2)]
        hg = A.alloc("hg", [128, HG_H, TS], F32)
        mix = A.alloc("mix", [128, HG_H, TS], BF16)
        junk = A.alloc("junk4", [128, D], BF16)
        ss = A.alloc("ss4", [128, 4], F32)
        rstd = A.alloc("rstd4", [128, 4], F32)
        xs = A.alloc("xs4", [128, 4, D], BF16)
        xnT = [A.alloc(f"xnT{j}", [128, 8, TS], BF16) for j in range(2)]
        mx = A.alloc("mx", [128, 4], F32)
        sm = A.alloc("smx", [128, 4], F32)
        ex = A.alloc("ex", [128, 4, NE], F32)
        for k in range(8):
            P.dma("pool", lambda e, k=k: e.dma_start(out=wo[:, k, :], in_=self.w_out[l, k * 128:(k + 1) * 128, :]),
                  w=[("wo", k)], key="wo")
        P.dma("pool", lambda e: e.dma_start(out=wr[:], in_=self.w_router[l].rearrange("(k p) n -> p k n", p=128)),
              w=["wr"], key="wr")
        for k in range(HG_H):
            P.dve(lambda e, k=k: e.tensor_scalar(out=wo[:, k, :], in0=wo[:, k, :], scalar1=self.hgw[:, l, k:k + 1],
                                                 scalar2=None, op0=ALU.mult),
                  r=[("wo", k), "hgw"], w=[("wo", k)])
        n2b = self.n2[:, l, :]

        def load(i):
            j = i % 2
            tsl = slice(i * TS, (i + 1) * TS)
            for d in range(2):
                P.dma("sp", lambda e, d=d: e.dma_start(out=OFt[j][:, d, :, :], in_=self.OF[:, d, :, tsl]),
                      r=[("OF", d, i)], w=[("OFt", j)], key=f"oft{j}")
            P.dma("sp", lambda e: e.dma_start(out=sgt[j][:], in_=self.HG[:, :, 5, tsl]),
                  r=[("HG", hd, i) for hd in range(HG_H)], w=[("sgt", j)], key=f"sgt{j}")
            P.dma("sp", lambda e: e.dma_start(out=att[j][:], in_=self.AO[:, :, tsl]), r=[("AO", i)], w=[("att", j)],
                  key=f"att{j}")
            P.dma("sp", lambda e: e.dma_start(
                out=xt[j][:], in_=self.XA[i * TS:(i + 1) * TS, :].rearrange("(a p) f -> p a f", p=128)),
                r=[("XA", i)], w=[("xt4", j)], key=f"xt4{j}")

        def body(i):
            j = i % 2
            tsl = slice(i * TS, (i + 1) * TS)
            x = xt[j]
            xn_ = ("xt4", j)
            P.dve(lambda e: e.tensor_tensor(out=ob[:], in0=OFt[j][:, 0, :, :], in1=OFt[j][:, 1, :, :], op=ALU.add),
                  r=[("OFt", j)], w=["ob"])
            P.act(lambda e: e.activation(out=sq[:], in_=ob[:], func=AF.Square), r=["ob"], w=["sq"])
            for hd in range(HG_H):
                b = hd % 2
                P.pe(lambda e, hd=hd, b=b: e.matmul(psF[b][:], self.ones_bf[:], sq[:, hd, :], start=True, stop=True),
                     r=["onesb", "sq"], w=[("psF", b)])
                lt = lnt[b]
                P.act(lambda e, b=b, lt=lt: e.activation(out=lt[:], in_=psF[b][:], func=AF.Ln, scale=1.0 / 128,
                                                         bias=self.epsb[:]),
                      r=[("psF", b), "epsb"], w=[("lnt", b)])
                P.act(lambda e, lt=lt: e.activation(out=lt[:], in_=lt[:], func=AF.Exp, scale=-0.5),
                      r=[("lnt", b)], w=[("lnt", b)])
                P.dve(lambda e, hd=hd, lt=lt: e.tensor_tensor(out=hg[:, hd, :], in0=ob[:, hd, :], in1=lt[:],
                                                              op=ALU.mult),
                      r=["ob", ("lnt", b)], w=[("hg", hd)])
                P.dve(lambda e, hd=hd: e.tensor_tensor(out=mix[:, hd, :], in0=hg[:, hd, :], in1=sgt[j][:, hd, :],
                                                       op=ALU.mult),
                      r=[("hg", hd), ("sgt", j)], w=[("mix", hd)])
            for a in range(4):
                for n in range(2):
                    b = 2 + (a * 2 + n) % 4
                    for k in range(8):
                        if k < HG_H:
                            lh, ln_ = mix[:, k, a * 128:(a + 1) * 128], ("mix", k)
                        else:
                            lh, ln_ = att[j][:, k - HG_H, a * 128:(a + 1) * 128], ("att", j)
                        P.pe(lambda e, b=b, k=k, n=n, lh=lh: e.matmul(psF[b][:], lh, wo[:, k, n * 512:(n + 1) * 512],
                                                                      start=(k == 0), stop=(k == 7)),
                             r=[ln_, ("wo", k)], w=[("psF", b)])
                    P.dve(lambda e, a=a, n=n, b=b: e.tensor_tensor(
                        out=x[:, a, n * 512:(n + 1) * 512], in0=x[:, a, n * 512:(n + 1) * 512], in1=psF[b][:],
                        op=ALU.add), r=[xn_, ("psF", b)], w=[xn_])
            P.dma("pool", lambda e: e.dma_start(
                out=self.XM[i * TS:(i + 1) * TS, :].rearrange("(a p) f -> p a f", p=128), in_=x[:]),
                r=[xn_], w=[("XM", i)], key=f"xmst{j}")
            P.dve(lambda e: e.memset(ss[:], 0.0), w=["ss4"])
            for a in range(4):
                P.act(lambda e, a=a: e.activation(out=junk[:], in_=x[:, a, :], func=AF.Square,
                                                  accum_out=ss[:, a:a + 1]), r=[xn_, "ss4"], w=["junk4", "ss4"])
            P.act(lambda e: e.activation(out=rstd[:], in_=ss[:], func=AF.Ln, scale=1.0 / D, bias=self.epsb[:]),
                  r=["ss4", "epsb"], w=["rstd4"])
            P.act(lambda e: e.activation(out=rstd[:], in_=rstd[:], func=AF.Exp, scale=-0.5), r=["rstd4"], w=["rstd4"])
            for a in range(4):
                P.dve(lambda e, a=a: e.tensor_scalar(out=xs[:, a, :], in0=x[:, a, :], scalar1=rstd[:, a:a + 1],
                                                     scalar2=None, op0=ALU.mult), r=[xn_, "rstd4"], w=[("xs4", a)])
            xT = xnT[j]
            for a in range(4):
                for k in range(8):
                    P.pe(lambda e, a=a, k=k: e.transpose(self.psBt[:, k * 128:(k + 1) * 128],
                                                         xs[:, a, k * 128:(k + 1) * 128], self.ident_bf[:]),
                         r=[("xs4", a), "identb"], w=[("psB", 0)])
                P.dve(lambda e, a=a: e.tensor_tensor(
                    out=xT[:, :, a * 128:(a + 1) * 128], in0=self.psBt[:].rearrange("p (k t) -> p k t", k=8),
                    in1=n2b.unsqueeze(2).to_broadcast([128, 8, 128]), op=ALU.mult),
                    r=[("psB", 0), "n2"], w=[("xnT", j)])
            P.dma("pool", lambda e: e.dma_start(out=self.XN[:, :, tsl], in_=xT[:]), r=[("xnT", j)], w=[("XN", i)],
                  key=f"xnst{j}")
            for a in range(4):
                for k in range(8):
                    P.pe(lambda e, a=a, k=k: e.matmul(psF[6][:, a * NE:(a + 1) * NE], xT[:, k, a * 128:(a + 1) * 128],
                                                      wr[:, k, :], start=(k == 0), stop=(k == 7)),
                         r=[("xnT", j), "wr"], w=[("psF", 6)])
            lg = psF[6][:, 0:4 * NE].rearrange("p (a n) -> p a n", a=4)
            P.dve(lambda e: e.tensor_reduce(out=mx[:], in_=lg, axis=AX.X, op=ALU.max), r=[("psF", 6)], w=["mx"])
            P.dve(lambda e: e.tensor_scalar(out=mx[:], in0=mx[:], scalar1=-1.0, scalar2=None, op0=ALU.mult),
                  r=["mx"], w=["mx"])
            P.dve(lambda e: e.memset(sm[:], 0.0), w=["smx"])
            for a in range(4):
                P.act(lambda e, a=a: e.activation(out=ex[:, a, :], in_=psF[6][:, a * NE:(a + 1) * NE], func=AF.Exp,
                                                  bias=mx[:, a:a + 1], accum_out=sm[:, a:a + 1]),
                      r=[("psF", 6), "mx", "smx"], w=["ex", "smx"])
            P.dve(lambda e: e.reciprocal(out=sm[:], in_=sm[:]), r=["smx"], w=["smx"])
            P.dve(lambda e: e.tensor_tensor(out=self.aff[:, i * 4:(i + 1) * 4, :], in0=ex[:],
                                            in1=sm[:].unsqueeze(2).to_broadcast([128, 4, NE]), op=ALU.mult),
                  r=["ex", "smx"], w=[("aff", i)])

        load(0)
        for i in range(self.nt):
            if i + 1 < self.nt:
                load(i + 1)
            body(i)

    def phase_p67(self, l, last):
        nc, P, A = self.nc, self.P, self.ph
        P.barrier()
        A.reset()
        psF = self.psF
        NBLK = self.nt * 4
        NTK = NBLK * 128
        CAPG = 2 * (4 * NTK) // NE
        Gt = A.alloc("Gt", [128, NBLK, NE], F32)
        mark = A.cur
        affT = A.alloc("affT", [NE, NTK], F32)
        G = A.alloc("G", [64, NTK], F32)
        sgn = A.alloc("sgn", [64, NTK], BF16)
        small = A.alloc("small", [64, 16], F32)
        gsum = A.alloc("gsum", [64, 64], F32)
        onesf = A.alloc("onesf", [64, 128], F32)
        Dm = A.alloc("Dm", [64, NE], F32)
        thr = A.alloc("thr", [128, NE], F32)
        for b4 in range(NBLK // 4):
            bank = b4 % 2
            for q in range(4):
                blk = b4 * 4 + q
                P.pe(lambda e, blk=blk, q=q, bank=bank: e.matmul(psF[bank][0:NE, q * 128:(q + 1) * 128],
                                                                 self.aff[:, blk, :], self.ident_f[:],
                                                                 start=True, stop=True),
                     r=[("aff", blk // 4), "identf"], w=[("psF", bank)])
            P.act(lambda e, b4=b4, bank=bank: e.copy(out=affT[:, b4 * 512:(b4 + 1) * 512], in_=psF[bank][0:NE, :]),
                  r=[("psF", bank)], w=["affT"])
        P.dma("sp", lambda e: e.dma_start(out=self.AFL[:, 0:NTK], in_=affT[:]), r=["affT"], w=["AFL"], key="afl")
        P.dma("pool", lambda e: e.collective_compute("AllGather", ALU.bypass, replica_groups=[[0, 1, 2, 3], [4, 5, 6, 7]],
                                                     ins=[self.AFL.opt()], outs=[self.AFG.opt()]),
              r=["AFL"], w=["AFG"], key="cc", inc=1)
        P.dma("sp", lambda e: e.dma_start(out=G[:], in_=self.AFG[:, 0:NTK]), r=["AFG"], w=["G"], key="gld")
        P.dma("sp", lambda e: e.dma_start(out=gsum[:], in_=self.c_gsum[:, :]), w=["gsum"], key="gsum")
        P.dma("sp", lambda e: e.dma_start(out=Dm[:], in_=self.c_dsel[:, :]), w=["Dm0"], key="dsel")
        P.dve(lambda e: e.memset(onesf[:], 1.0), w=["onesf"])
        P.dve(lambda e: e.memset(small[:], 0.0), w=["small"])
        P.dve(lambda e: e.memset(small[:, 1:2], 1.0), r=["small"], w=["small"])
        P.dve(lambda e: e.memset(small[:, 2:3], -0.5), r=["small"], w=["small"])
        LO, HI, NM, CNT, TOT, MM, DD, D2, SS = (slice(j, j + 1) for j in range(9))
        target = float(2 * CAPG - 4 * NTK)
        for it in range(26):
            P.dve(lambda e: e.memset(small[:, CNT], 0.0), r=["small"], w=["small"])
            P.act(lambda e: e.activation(out=sgn[:], in_=G[:], func=AF.Sign, bias=small[:, NM],
                                         accum_out=small[:, CNT]), r=["G", "small"], w=["sgn", "small"])
            P.pe(lambda e: e.matmul(psF[2][0:64, 0:1], gsum[:], small[:, CNT], start=True, stop=True),
                 r=["gsum", "small"], w=[("psF", 2)])
            P.dve(lambda e: e.tensor_scalar(out=small[:, MM], in0=psF[2][0:64, 0:1], scalar1=target, scalar2=None,
                                            op0=ALU.is_ge), r=[("psF", 2)], w=["small"])
            P.dve(lambda e: e.scalar_tensor_tensor(out=small[:, DD], in0=small[:, NM], scalar=-1.0, in1=small[:, LO],
                                                   op0=ALU.mult, op1=ALU.subtract), r=["small"], w=["small"])
            P.dve(lambda e: e.tensor_tensor(out=small[:, D2], in0=small[:, HI], in1=small[:, NM], op=ALU.add),
                  r=["small"], w=["small"])
            P.dve(lambda e: e.scalar_tensor_tensor(out=small[:, LO], in0=small[:, DD], scalar=small[:, MM],
                                                   in1=small[:, LO], op0=ALU.mult, op1=ALU.add),
                  r=["small"], w=["small"])
            P.dve(lambda e: e.scalar_tensor_tensor(out=small[:, HI], in0=small[:, D2], scalar=small[:, MM],
                                                   in1=small[:, NM], op0=ALU.mult, op1=ALU.subtract),
                  r=["small"], w=["small"])
            P.dve(lambda e: e.tensor_tensor(out=small[:, SS], in0=small[:, LO], in1=small[:, HI], op=ALU.add),
                  r=["small"], w=["small"])
            P.dve(lambda e: e.tensor_scalar(out=small[:, NM], in0=small[:, SS], scalar1=-0.5, scalar2=None,
                                            op0=ALU.mult), r=["small"], w=["small"])
        P.dve(lambda e: e.tensor_scalar(out=Dm[:], in0=Dm[:], scalar1=small[:, LO], scalar2=None, op0=ALU.mult),
              r=["Dm0", "small"], w=["Dm"])
        P.pe(lambda e: e.matmul(psF[3][:, 0:NE], onesf[:], Dm[:], start=True, stop=True), r=["onesf", "Dm"],
             w=[("psF", 3)])
        P.act(lambda e: e.copy(out=thr[:], in_=psF[3][:, 0:NE]), r=[("psF", 3)], w=["thr"])
        affv = self.aff[:, 0:NBLK, :]
        P.dve(lambda e: e.tensor_tensor(out=Gt[:], in0=affv, in1=thr[:].unsqueeze(1).to_broadcast([128, NBLK, NE]),
                                        op=ALU.is_ge), r=[("aff", i) for i in range(self.nt)] + ["thr"], w=["Gt"])
        P.dve(lambda e: e.tensor_tensor(out=Gt[:], in0=Gt[:], in1=affv, op=ALU.mult),
              r=[("aff", i) for i in range(self.nt)] + ["Gt"], w=["Gt"])

        if "GT" in self.debug:
            P.dma("sp", lambda e: e.dma_start(out=self.GTd[:, :, :], in_=Gt[:]), r=["Gt"], w=["GTd"], key="gtd")
            P.dma("sp", lambda e: e.dma_start(out=self.THd[:, :], in_=small[:]), r=["small"], w=["THd"], key="thd")
        P.barrier()
        A.cur = mark
        NEX = self.nexp
        STT = 1024
        NST = NTK // STT
        W = [[A.alloc(f"W{nm}{j}", [128, 8, D], BF16) for j in range(2)] for nm in range(3)]
        xnT = A.alloc("xnT7", [128, 8, STT], BF16)
        acc = A.alloc("acc", [128, 8, D], F32)
        hT = A.alloc("hT7", [128, 8, TS], BF16)
        sg = [A.alloc(f"sg7{j}", [128, TS], BF16) for j in range(2)]
        xm1 = A.alloc("xm70", [128, D], F32)
        xm = [xm1, xm1]
        junk = hT[:].rearrange("p k t -> p (k t)")[:, 0:D]
        ss = A.alloc("ss7", [128, 8], F32)
        rstd = A.alloc("rstd7", [128, 8], F32)
        fnw = A.alloc("fnw", [128, D], F32)
        if last:
            P.dma("sp", lambda e: e.dma_start(out=fnw[:], in_=self.c_fnw.to_broadcast([128, D])), w=["fnw"], key="fnw")
        wsrc = (self.w_gate, self.w_up, self.w_down)
        cnt = {"w": 0, "g": 0, "d": 0}

        def loadw(e_):
            j = cnt["w"] % 2
            cnt["w"] += 1
            for nm in range(3):
                for kh in range(2):
                    P.dma("pool", lambda e, nm=nm, kh=kh: e.dma_start(
                        out=W[nm][j][:, kh * 4:(kh + 1) * 4, :],
                        in_=wsrc[nm][l, e_, kh * 512:(kh + 1) * 512, :].rearrange("(k p) f -> p k f", p=128)),
                        w=[("W", nm, j)], key=f"w{nm}{j}")
            return j

        def expert(st, e_, j):
            Wg, Wu, Wd = W[0][j], W[1][j], W[2][j]
            for tl in range(STT // TS):
                for m in range(8):
                    gi = cnt["g"] % 2
                    cnt["g"] += 1
                    bG, bU = gi, 2 + gi
                    for k in range(8):
                        P.pe(lambda e, k=k, m=m, bG=bG, tl=tl: e.matmul(psF[bG][:], Wg[:, k, m * 128:(m + 1) * 128],
                                                                 xnT[:, k, tl * TS:(tl + 1) * TS],
                                                                 start=(k == 0), stop=(k == 7)),
                             r=[("W", 0, j), "xnT7"], w=[("psF", bG)])
                    for k in range(8):
                        P.pe(lambda e, k=k, m=m, bU=bU, tl=tl: e.matmul(psF[bU][:], Wu[:, k, m * 128:(m + 1) * 128],
                                                                 xnT[:, k, tl * TS:(tl + 1) * TS],
                                                                 start=(k == 0), stop=(k == 7)),
                             r=[("W", 1, j), "xnT7"], w=[("psF", bU)])
                    P.act(lambda e, gi=gi, bG=bG: e.activation(out=sg[gi][:], in_=psF[bG][:], func=AF.Silu),
                          r=[("psF", bG)], w=[("sg7", gi)])
                    P.dve(lambda e, gi=gi, bU=bU, m=m: e.tensor_tensor(out=hT[:, m, :], in0=psF[bU][:], in1=sg[gi][:],
                                                                       op=ALU.mult),
                          r=[("psF", bU), ("sg7", gi)], w=[("hT7", m)])
                for sub in range(4):
                    s8 = tl * 4 + sub
                    blk = st * 8 + s8
                    gate = Gt[:, blk, e_:e_ + 1]
                    for n in range(2):
                        bD = 4 + cnt["d"] % 3
                        cnt["d"] += 1
                        for k in range(8):
                            P.pe(lambda e, k=k, n=n, bD=bD, sub=sub: e.matmul(
                                psF[bD][:], hT[:, k, sub * 128:(sub + 1) * 128], Wd[:, k, n * 512:(n + 1) * 512],
                                start=(k == 0), stop=(k == 7)),
                                r=[("hT7", k), ("W", 2, j)], w=[("psF", bD)])
                        asl = acc[:, s8, n * 512:(n + 1) * 512]
                        if e_ == 0:
                            P.dve(lambda e, bD=bD, asl=asl, gate=gate: e.tensor_scalar(
                                out=asl, in0=psF[bD][:], scalar1=gate, scalar2=None, op0=ALU.mult),
                                r=[("psF", bD), "Gt"], w=[("acc", s8)])
                        else:
                            P.dve(lambda e, bD=bD, asl=asl, gate=gate: e.scalar_tensor_tensor(
                                out=asl, in0=psF[bD][:], scalar=gate, in1=asl, op0=ALU.mult, op1=ALU.add),
                                r=[("psF", bD), "Gt", ("acc", s8)], w=[("acc", s8)])

        def finish(st):
            for s8 in range(8):
                xj = 0
                row0 = st * STT + s8 * 128
                P.dma("sp", lambda e, xj=xj, row0=row0: e.dma_start(out=xm[xj][:], in_=self.XM[row0:row0 + 128, :]),
                      r=[("XM", row0 // TS)], w=[("xm7", xj)], key=f"xm7{xj}")
                P.dve(lambda e, s8=s8, xj=xj: e.tensor_tensor(out=acc[:, s8, :], in0=acc[:, s8, :], in1=xm[xj][:],
                                                              op=ALU.add),
                      r=[("acc", s8), ("xm7", xj)], w=[("acc", s8)])
            accn = [("acc", s8) for s8 in range(8)]
            if not last:
                P.dma("pool", lambda e: e.dma_start(
                    out=self.XA[st * STT:(st + 1) * STT, :].rearrange("(a p) f -> p a f", p=128), in_=acc[:]),
                    r=accn, w=[("XA", 2 * st), ("XA", 2 * st + 1)], key="xast")
            else:
                P.dve(lambda e: e.memset(ss[:], 0.0), w=["ss7"])
                for s8 in range(8):
                    P.act(lambda e, s8=s8: e.activation(out=junk, in_=acc[:, s8, :], func=AF.Square,
                                                        accum_out=ss[:, s8:s8 + 1]),
                          r=[("acc", s8), "ss7"], w=[("hT7", k) for k in range(8)] + ["ss7"])
                P.act(lambda e: e.activation(out=rstd[:], in_=ss[:], func=AF.Ln, scale=1.0 / D, bias=self.epsb[:]),
                      r=["ss7", "epsb"], w=["rstd7"])
                P.act(lambda e: e.activation(out=rstd[:], in_=rstd[:], func=AF.Exp, scale=-0.5),
                      r=["rstd7"], w=["rstd7"])
                for s8 in range(8):
                    P.dve(lambda e, s8=s8: e.scalar_tensor_tensor(
                        out=acc[:, s8, :], in0=acc[:, s8, :], scalar=rstd[:, s8:s8 + 1], in1=fnw[:],
                        op0=ALU.mult, op1=ALU.mult), r=[("acc", s8), "rstd7", "fnw"], w=[("acc", s8)])
                P.dma("pool", lambda e: e.dma_start(
                    out=self.y_out[st * STT:(st + 1) * STT, :].rearrange("(a p) f -> p a f", p=128), in_=acc[:]),
                    r=accn, w=[("Y", st)], key="yst")

        pend = loadw(0)
        for st in range(NST):
            P.dma("sp", lambda e, st=st: e.dma_start(out=xnT[:], in_=self.XN[:, :, st * STT:(st + 1) * STT]),
                  r=[("XN", 2 * st), ("XN", 2 * st + 1)], w=["xnT7"], key="xn7")
            for e_ in range(NEX):
                j = pend
                nxt = (st, e_ + 1) if e_ + 1 < NEX else ((st + 1, 0) if st + 1 < NST else None)
                if nxt is not None:
                    pend = loadw(nxt[1])
                expert(st, e_, j)
            finish(st)

def _rope_tables(pos):
    half = 8
    inv_freq = (np.float32(ROPE_THETA) ** (-np.arange(half, dtype=np.float32) * np.float32(2.0) / np.float32(16)))
    inv_freq = inv_freq.astype(np.float32)
    ang = pos.astype(np.float32)[:, None] * inv_freq[None, :]
    cos = np.cos(ang.astype(np.float64)).astype(np.float32)
    sin = np.sin(ang.astype(np.float64)).astype(np.float32)
    C = np.ones((64, pos.shape[0]), np.float32)
    S = np.zeros((64, pos.shape[0]), np.float32)
    C[0:8] = cos.T
    C[8:16] = cos.T
    S[0:8] = -sin.T
    S[8:16] = sin.T
    return np.concatenate([C, C], 0), np.concatenate([S, S], 0)


def _core_consts(core):
    seqlen = NTOK if core < 4 else 2048
    pos = np.arange(NTOK) % seqlen
    C, S = _rope_tables(pos)
    flags = np.ones((128, 2, NCHUNK), np.float32)
    for c in range(NCHUNK):
        if (c * CH) % seqlen == 0:
            flags[:, 0, c] = 0.0
        if ((c + 1) * CH) % seqlen == 0:
            flags[:, 1, c] = 0.0
    kk = np.arange(128)[:, None]
    qq = np.arange(128)[None, :]
    m1 = np.where(kk >= qq, 0.0, MASKNEG)
    m2 = np.where(kk <= qq, 0.0, MASKNEG)
    m1e = np.where((kk >= qq) & (kk >= 64), 0.0, MASKNEG)
    m2e = np.where((kk <= qq) & (kk < 64), 0.0, MASKNEG)
    am = np.zeros((128, 3, 2, 128), np.float32)
    am[:, 0, 0], am[:, 0, 1] = m1, m2
    am[:, 1, 0], am[:, 1, 1] = m1e, m2e
    if core < 4:
        am[:, 2, 0], am[:, 2, 1] = m1, m2
    else:
        am[:, 2, 0], am[:, 2, 1] = m1e, m2e
    return dict(c_ropeC=C, c_ropeS=S, c_flags=flags, c_amask=am.astype(np.float32))


def _shared_consts(inp):
    f32 = np.float32
    w_in = np.asarray(inp["w_in"], f32)
    def rotperm(w):
        w = w.reshape(DEPTH, D, 6, 64)
        o = np.zeros_like(w)
        o[..., 0:8] = w[..., 8:16]
        o[..., 8:16] = w[..., 0:8]
        return o.reshape(DEPTH, D, 384)
    qa = w_in[:, :, 3200:3584]
    ka = w_in[:, :, 3584:3968]
    w_ext = np.concatenate([w_in, rotperm(qa), rotperm(ka)], axis=2)
    def fm(v, n):
        return np.ascontiguousarray(np.asarray(v, f32).reshape(DEPTH, n, 128).transpose(2, 0, 1))
    lbl = np.stack([np.asarray(inp["lb_fwd_logits"], f32), np.asarray(inp["lb_bwd_logits"], f32)], 0)
    lbl = np.ascontiguousarray(lbl.reshape(2, DEPTH, HG_H, 128).transpose(3, 0, 2, 1))
    rst = np.ones((128, TS), f32)
    rst[:, ::CH] = 0.0
    s = np.arange(CH)[:, None]
    t = np.arange(CH)[None, :]
    hmask = np.zeros((CH, 2, CH), f32)
    hmask[:, 0, :] = np.where(s <= t, -1.0, 0.0)
    hmask[:, 1, :] = np.where(s >= t, -1.0, 0.0)
    return dict(
        w_in=np.ascontiguousarray(w_ext),
        w_out=np.asarray(inp["w_out"], f32), w_router=np.asarray(inp["w_router"], f32),
        w_gate=np.asarray(inp["w_gate"], f32), w_up=np.asarray(inp["w_up"], f32),
        w_down=np.asarray(inp["w_down"], f32),
        c_n1=fm(inp["norm1_w"], 8), c_n2=fm(inp["norm2_w"], 8), c_hgw=fm(inp["hgrn_norm_w"], HG_H),
        c_fnw=np.asarray(inp["final_norm_w"], f32).reshape(1, D),
        c_lbl=lbl, c_ident=np.eye(128, dtype=f32), c_rst=rst, c_hmask=hmask,
        c_gsum=(np.arange(64)[:, None] % NE == np.arange(64)[None, :] % NE).astype(f32),
        c_dsel=(np.arange(64)[:, None] == np.arange(NE)[None, :]).astype(f32),
    )


def make_in_maps(inp):
    shared = _shared_consts(inp)
    xp = np.asarray(inp["x_prompt"], np.float32)
    xsm = np.asarray(inp["x_sample"], np.float32)
    maps = []
    for c in range(NCORES):
        m = dict(shared)
        if c < 4:
            m["x"] = np.ascontiguousarray(xp[c])
        else:
            m["x"] = np.ascontiguousarray(xsm[(c - 4) * 4:(c - 3) * 4].reshape(NTOK, D))
        m.update(_core_consts(c))
        maps.append(m)
    return maps


def kernel(**inputs):
    b = Builder()
    nc = b.build()
    maps = make_in_maps(inputs)
    res = run_bass_kernel_spmd(nc, maps, core_ids=list(range(NCORES)))
    ys = [np.asarray(r["y"], np.float32) for r in res.results]
    y_prompt = np.stack(ys[0:4], 0)
    y_sample = np.concatenate([y.reshape(4, 2048, D) for y in ys[4:8]], 0)
    return (y_prompt, y_sample)
```

```python
import math
from contextlib import ExitStack

import numpy as np
import ml_dtypes

import concourse.bass as bass
import concourse.mybir as mybir
from concourse.bass_utils import run_bass_kernel_spmd

F32 = mybir.dt.float32
BF16 = mybir.dt.bfloat16
I32 = mybir.dt.int32
AF = mybir.ActivationFunctionType
ALU = mybir.AluOpType
AX = mybir.AxisListType

NCORES = 8
D = 1024
DEPTH = 4
NTOK = 8192
TS = 512
NT = NTOK // TS
HG_H = 5
CH = 64
NCHUNK = NTOK // CH
NE = 16
EPS = 1e-6
ROPE_THETA = 500000.0
WA = 3200
WB = 1920
SCALE_Q = 0.125
MASKNEG = -30000.0

ENGS = ("pe", "act", "dve", "pool", "sp")


class Op:
    __slots__ = ("eng", "fn", "r", "w", "dma", "key", "idx", "sig", "n", "bar", "inc", "ep")

    def __init__(self, eng, fn, r, w, dma, key):
        self.eng = eng
        self.fn = fn
        self.r = r
        self.w = w
        self.dma = dma
        self.key = key
        self.sig = False
        self.n = 0
        self.bar = False
        self.inc = 16


class Prog:
    def __init__(self, nc):
        self.nc = nc
        self.ops = []
        self.epoch = 0

    def new_epoch(self):
        self.epoch += 1

    def add(self, eng, fn, r=(), w=(), dma=False, key=None):
        op = Op(eng, fn, tuple(r), tuple(w), dma, key)
        op.ep = self.epoch
        op.idx = len(self.ops)
        self.ops.append(op)
        return op

    def pe(self, fn, r=(), w=()):
        return self.add("pe", fn, r, w)

    def act(self, fn, r=(), w=()):
        return self.add("act", fn, r, w)

    def dve(self, fn, r=(), w=()):
        return self.add("dve", fn, r, w)

    def pool(self, fn, r=(), w=()):
        return self.add("pool", fn, r, w)

    def dma(self, eng, fn, r=(), w=(), key=None, inc=16):
        assert key is not None
        op = self.add(eng, fn, r, w, dma=True, key=key)
        op.inc = inc
        return op

    def barrier(self):
        for e in ENGS:
            op = self.add(e, None)
            op.bar = True

    def finalize(self, stack):
        nc = self.nc
        ops = self.ops
        last_w = {}
        readers = {}
        deps = [None] * len(ops)
        last_on = {e: None for e in ENGS}
        bar_waits = {}
        for op in ops:
            if op.bar:
                bar_waits[op.idx] = dict(last_on)
                continue
            d = set()
            for b in op.r:
                j = last_w.get(b)
                if j is not None:
                    d.add(j)
            for b in op.w:
                j = last_w.get(b)
                if j is not None:
                    d.add(j)
                rs = readers.get(b)
                if rs:
                    d.update(rs.values())
            for b in op.r:
                rk = ("dma", op.idx) if op.dma else op.eng
                readers.setdefault(b, {})[rk] = op.idx
            for b in op.w:
                last_w[b] = op.idx
                readers[b] = {}
            d.discard(op.idx)
            deps[op.idx] = d
            if not op.dma:
                last_on[op.eng] = op.idx
        for op in ops:
            if op.bar:
                for e, j in bar_waits[op.idx].items():
                    if j is not None and e != op.eng:
                        ops[j].sig = True
                continue
            keep = []
            for j in deps[op.idx]:
                pj = ops[j]
                if (not pj.dma) and (not op.dma) and pj.eng == op.eng:
                    if op.eng == "pe":
                        continue
                    raw = any(b in pj.w for b in op.r) or any(b in pj.w for b in op.w)
                    if not raw:
                        continue
                keep.append(j)
                if not pj.dma:
                    pj.sig = True
            deps[op.idx] = keep
        cnt = {}
        keycnt = {}
        for op in ops:
            if op.bar:
                continue
            if op.dma:
                keycnt[op.key] = keycnt.get(op.key, 0) + 1
                op.n = keycnt[op.key]
            elif op.sig:
                ck = (op.eng, op.ep)
                cnt[ck] = cnt.get(ck, 0) + 1
                op.n = cnt[ck]
        keycnt2 = {}
        waits_for = [None] * len(ops)
        waited = {e: {} for e in ENGS}
        for op in ops:
            w = {}
            if op.bar:
                for e, j in bar_waits[op.idx].items():
                    if j is not None and e != op.eng:
                        w[("e", e, ops[j].ep)] = ops[j].n
                for k, c in keycnt2.items():
                    w[("k", k)] = c
            else:
                for j in deps[op.idx]:
                    pj = ops[j]
                    if pj.dma:
                        sem = ("k", pj.key)
                        val = keycnt2.get(pj.key, 0)
                    else:
                        sem = ("e", pj.eng, pj.ep)
                        val = pj.n
                    if val > w.get(sem, 0):
                        w[sem] = val
            wl = []
            for sem, val in w.items():
                if val <= 0 or waited[op.eng].get(sem, 0) >= val:
                    continue
                waited[op.eng][sem] = val
                wl.append((sem, val))
            waits_for[op.idx] = wl
            if op.dma:
                keycnt2[op.key] = keycnt2.get(op.key, 0) + op.inc
        sems = {}
        for (e, ep), c in cnt.items():
            sems[("e", e, ep)] = stack.enter_context(nc.semaphore(f"c_{e}{ep}"))
        for k in keycnt2:
            sems[("k", k)] = stack.enter_context(nc.semaphore("k_" + str(k)))
        self.nsems = len(sems)
        self.sig_counts = cnt
        block = stack.enter_context(nc.Block())
        per = {e: [op for op in ops if op.eng == e] for e in ENGS}

        def emit(eng_name, engine):
            for op in per[eng_name]:
                for sem, val in waits_for[op.idx]:
                    engine.wait_ge(sems[sem], val)
                if op.bar:
                    continue
                ins = op.fn(engine)
                if op.dma:
                    ins.then_inc(sems[("k", op.key)], op.inc)
                elif op.sig:
                    ins.then_inc(sems[("e", op.eng, op.ep)], 1)
            if eng_name == "sp":
                for k, c in keycnt2.items():
                    engine.wait_ge(sems[("k", k)], c)
                for (e, ep), c in cnt.items():
                    engine.wait_ge(sems[("e", e, ep)], c)

        @block.tensor
        def _(e):
            emit("pe", e)

        @block.scalar
        def _(e):
            emit("act", e)

        @block.vector
        def _(e):
            emit("dve", e)

        @block.gpsimd
        def _(e):
            emit("pool", e)

        @block.sync
        def _(e):
            emit("sp", e)


class Arena:
    def __init__(self, nc, lo, hi, tag):
        self.nc, self.lo, self.hi, self.cur, self.n, self.tag = nc, lo, hi, lo, 0, tag

    def reset(self):
        self.cur = self.lo

    def alloc(self, name, shape, dtype):
        esz = 4 if dtype in (F32, I32) else 2
        size = esz
        for s in shape[1:]:
            size *= s
        size = (size + 31) // 32 * 32
        off = self.cur
        self.cur += size
        assert self.cur <= self.hi, (name, self.cur, self.hi)
        self.n += 1
        return self.nc.alloc_sbuf_tensor_at(f"{self.tag}{self.n}_{name}", list(shape), dtype, offset=off)


class Builder:
    def __init__(self, nlayers=DEPTH, debug=None, stop=None, nt=NT, nexp=NE):
        self.nlayers = nlayers
        self.nt = nt
        self.nexp = nexp
        self.sparse = True
        self.debug = debug or ()
        self.stop = stop
        self.nc = bass.Bass("TRN2", target_bir_lowering=False)
        self.P = Prog(self.nc)
        self.pers = Arena(self.nc, 18 * 1024, 47 * 1024, "p")
        self.ph = Arena(self.nc, 47 * 1024, 223 * 1024, "t")
        self.uid = 0

    def din(self, name, shape, dtype=F32):
        return self.nc.dram_tensor(name, list(shape), dtype, kind="ExternalInput").ap()

    def dscr(self, name, shape, dtype):
        kind = "ExternalOutput" if name in self.debug else "Internal"
        return self.nc.dram_tensor(name, list(shape), dtype, kind=kind).ap()

    def build(self):
        nc, P = self.nc, self.P
        with ExitStack() as st:
            self.st = st
            self.declare()
            self.setup_consts()
            for l in range(self.nlayers):
                self.layer(l)
                if self.stop is not None and l == self.nlayers - 1:
                    break
            P.barrier()
            P.finalize(st)
        return nc

    def declare(self):
        nc = self.nc
        self.x_in = self.din("x", [NTOK, D])
        self.y_out = nc.dram_tensor("y", [NTOK, D], F32, kind="ExternalOutput").ap()
        self.w_in = self.din("w_in", [DEPTH, D, WA + WB])
        self.w_out = self.din("w_out", [DEPTH, D, D])
        self.w_router = self.din("w_router", [DEPTH, D, NE])
        ne_decl = NE if self.stop is None else 1
        self.w_gate = self.din("w_gate", [DEPTH, ne_decl, D, D])
        self.w_up = self.din("w_up", [DEPTH, ne_decl, D, D])
        self.w_down = self.din("w_down", [DEPTH, ne_decl, D, D])
        self.c_n1 = self.din("c_n1", [128, DEPTH, 8])
        self.c_n2 = self.din("c_n2", [128, DEPTH, 8])
        self.c_hgw = self.din("c_hgw", [128, DEPTH, HG_H])
        self.c_fnw = self.din("c_fnw", [1, D])
        self.c_lbl = self.din("c_lbl", [128, 2, HG_H, DEPTH])
        self.c_ropeC = self.din("c_ropeC", [128, NTOK])
        self.c_ropeS = self.din("c_ropeS", [128, NTOK])
        self.c_ident = self.din("c_ident", [128, 128])
        self.c_rst = self.din("c_rst", [128, TS])
        self.c_hmask = self.din("c_hmask", [CH, 2, CH])
        self.c_flags = self.din("c_flags", [128, 2, NCHUNK])
        self.c_amask = self.din("c_amask", [128, 3, 2, 128])
        self.XA = self.dscr("XA", [NTOK, D], F32)
        self.XM = self.dscr("XM", [NTOK, D], F32)
        self.HT = self.dscr("HT", [128, 8, NTOK], BF16)
        self.HG = self.dscr("HG", [128, HG_H, 6, NTOK], BF16)
        self.AT = self.dscr("AT", [128, 9, NTOK], BF16)
        self.OF = self.dscr("OF", [128, 2, HG_H, NTOK], BF16)
        self.AO = self.dscr("AO", [128, 3, NTOK], BF16)
        self.XN = self.dscr("XN", [128, 8, NTOK], BF16)
        if "GT" in self.debug:
            self.GTd = self.nc.dram_tensor("GTd", [128, NTOK // 128, NE], F32, kind="ExternalOutput").ap()
            self.THd = self.nc.dram_tensor("THd", [64, 16], F32, kind="ExternalOutput").ap()
        self.AFL = self.dscr("AFL", [NE, NTOK], F32)
        self.AFG = self.dscr("AFG", [4 * NE, NTOK], F32)
        self.XNK = self.dscr("XNK", [NTOK, D], BF16)
        self.c_tri = self.din("c_tri", [128, 128])
        self.c_iota = self.din("c_iota", [128, 4, 128])
        self.c_n2row = self.din("c_n2row", [DEPTH, D])
        self.c_gsum = self.din("c_gsum", [64, 64])
        self.c_dsel = self.din("c_dsel", [64, NE])

    def setup_consts(self):
        nc, P, A = self.nc, self.P, self.pers
        self.ident_bf = A.alloc("identb", [128, 128], BF16)
        self.ident_f = A.alloc("identf", [128, 128], F32)
        self.ones_bf = A.alloc("onesb", [128, 128], BF16)
        self.n1 = A.alloc("n1", [128, DEPTH, 8], F32)
        self.n2 = A.alloc("n2", [128, DEPTH, 8], F32)
        self.hgw = A.alloc("hgw", [128, DEPTH, HG_H], F32)
        self.lb = A.alloc("lb", [128, 2, HG_H, DEPTH], F32)
        self.oml = A.alloc("oml", [128, 2, HG_H, DEPTH], F32)
        self.rst = A.alloc("rst", [128, TS], F32)
        self.hmask = A.alloc("hmask", [CH, 2, CH], F32)
        self.flags = A.alloc("flags", [128, 2, NCHUNK], F32)
        self.scal = A.alloc("scal", [128, 2, HG_H, 3, NCHUNK], F32)
        self.aff = A.alloc("aff", [128, NTOK // 128, NE], F32)
        self.amask = A.alloc("amask", [128, 3, 2, 128], BF16)
        self.epsb = A.alloc("epsb", [128, 1], F32)
        P.dve(lambda e: e.memset(self.epsb[:], EPS), w=["epsb"])
        self.psF = [self.st.enter_context(nc.psum_tensor(f"psF{j}", [128, 512], F32)) for j in range(7)]
        psb = self.st.enter_context(nc.psum_tensor("psB", [128, 1024], BF16))
        self.psB = [psb, psb]
        self.psBt = psb
        lbt = A.alloc("lbt", [128, 2, HG_H, DEPTH], F32)
        lbs = A.alloc("lbs", [128, 2, HG_H, 1], F32)

        def ld(dst, src, name, eng="sp"):
            P.dma(eng, lambda e: e.dma_start(out=dst, in_=src), w=[name], key="c_" + name)

        ld(self.ident_f[:], self.c_ident[:, :], "identf")
        ld(self.ident_bf[:], self.c_ident[:, :], "identb", "pool")
        ld(self.n1[:], self.c_n1[:, :, :], "n1")
        ld(self.n2[:], self.c_n2[:, :, :], "n2")
        ld(self.hgw[:], self.c_hgw[:, :, :], "hgw")
        ld(lbt[:], self.c_lbl[:, :, :, :], "lbt")
        ld(self.rst[:], self.c_rst[:, :], "rst")
        ld(self.hmask[:], self.c_hmask[:, :, :], "hmask")
        ld(self.flags[:], self.c_flags[:, :, :], "flags")
        ld(self.amask[:], self.c_amask[:, :, :, :], "amask", "pool")
        P.dve(lambda e: e.memset(self.ones_bf[:], 1.0), w=["onesb"])
        P.act(lambda e: e.activation(out=lbt[:], in_=lbt[:], func=AF.Exp), r=["lbt"], w=["lbt"])
        P.dve(lambda e: e.tensor_reduce(out=lbs[:], in_=lbt[:], axis=AX.X, op=ALU.add), r=["lbt"], w=["lbs"])
        P.dve(lambda e: e.reciprocal(out=lbs[:], in_=lbs[:]), r=["lbs"], w=["lbs"])
        P.dve(lambda e: e.tensor_tensor(out=lbt[:], in0=lbt[:], in1=lbs[:].to_broadcast([128, 2, HG_H, DEPTH]),
                                        op=ALU.mult), r=["lbt", "lbs"], w=["lbt"])
        P.dve(lambda e: e.memset(self.lb[:, :, :, 0:1], 0.0), w=["lb"])
        for l in range(1, DEPTH):
            P.dve(lambda e, l=l: e.tensor_tensor(out=self.lb[:, :, :, l:l + 1], in0=self.lb[:, :, :, l - 1:l],
                                                 in1=lbt[:, :, :, l:l + 1], op=ALU.add),
                  r=["lbt", "lb"], w=["lb"])
        P.dve(lambda e: e.tensor_scalar(out=self.oml[:], in0=self.lb[:], scalar1=-1.0, scalar2=1.0,
                                        op0=ALU.mult, op1=ALU.add), r=["lb"], w=["oml"])
        for i in range(4):
            sl = slice(i * 2048, (i + 1) * 2048)
            P.dma("sp", lambda e, sl=sl: e.dma_start(out=self.XA[sl, :], in_=self.x_in[sl, :]),
                  w=[("XA", j) for j in range(i * 4, i * 4 + 4)], key="xcopy")


    def layer(self, l):
        self.phase_p1a(l)
        if self.stop == "p1a":
            return
        self.phase_p1b(l)
        if self.stop == "p1b":
            return
        self.phase_p2(l)
        if self.stop == "p2":
            return
        self.phase_p3(l)
        if self.stop == "p3":
            return
        self.phase_p4(l)
        if self.stop == "p4":
            return
        self.P.new_epoch()
        self.phase_p67(l, last=(l == self.nlayers - 1))
        self.P.new_epoch()

    def phase_p1a(self, l):
        nc, P, A = self.nc, self.P, self.ph
        P.barrier()
        A.reset()
        st = self.st
        win = A.alloc("win", [128, 8, WA], BF16)
        xt = A.alloc("xt", [128, 4, D], F32)
        xs = A.alloc("xs", [128, 4, D], BF16)
        junk = A.alloc("junk", [128, D], BF16)
        ss = A.alloc("ss", [128, 4], F32)
        rstd = A.alloc("rstd", [128, 4], F32)
        hT = [A.alloc(f"hT{j}", [128, 8, TS], BF16) for j in range(2)]
        OH = [A.alloc(f"OH{j}", [128, 6, TS], BF16) for j in range(2)]
        NSET = 2
        T = []
        for j in range(NSET):
            d = {}
            for nm in ("eq", "qsb", "eg", "gsb"):
                d[nm] = A.alloc(f"{nm}{j}", [128, TS], F32)
            for dr in range(2):
                for nm in ("t0", "t1", "b", "E1"):
                    d[(nm, dr)] = A.alloc(f"{nm}{j}{dr}", [128, TS], F32)
                d[("sm", dr)] = A.alloc(f"sm{j}{dr}", [128, 3, 8], F32)
            T.append(d)
        psF, psB = self.psF, self.psB

        for k in range(8):
            P.dma("pool", lambda e, k=k: e.dma_start(out=win[:, k, :], in_=self.w_in[l, k * 128:(k + 1) * 128, 0:WA]),
                  w=[("win", k)], key="win")
        n1b = self.n1[:, l, :]

        def norm_tile(i):
            tsl = slice(i * TS, (i + 1) * TS)
            P.dma("sp", lambda e: e.dma_start(
                out=xt[:], in_=self.XA[i * TS:(i + 1) * TS, :].rearrange("(a p) f -> p a f", p=128)),
                r=[("XA", i)], w=["xt"], key="xt")
            P.dve(lambda e: e.memset(ss[:], 0.0), w=["ss"])
            for a in range(4):
                P.act(lambda e, a=a: e.activation(out=junk[:], in_=xt[:, a, :], func=AF.Square,
                                                  accum_out=ss[:, a:a + 1]),
                      r=["xt", "ss"], w=["junk", "ss"])
            P.act(lambda e: e.activation(out=rstd[:], in_=ss[:], func=AF.Ln, scale=1.0 / D, bias=self.epsb[:]),
                  r=["ss", "epsb"], w=["rstd"])
            P.act(lambda e: e.activation(out=rstd[:], in_=rstd[:], func=AF.Exp, scale=-0.5), r=["rstd"], w=["rstd"])
            for a in range(4):
                P.dve(lambda e, a=a: e.tensor_scalar(out=xs[:, a, :], in0=xt[:, a, :], scalar1=rstd[:, a:a + 1],
                                                     scalar2=None, op0=ALU.mult),
                      r=["xt", "rstd"], w=[("xs", a)])
            h = hT[i % 2]
            hname = ("hT", i % 2)
            for a in range(4):
                pb = psB[0]
                pbn = ("psB", 0)
                for k in range(8):
                    P.pe(lambda e, a=a, k=k, pb=pb: e.transpose(pb[:, k * 128:(k + 1) * 128],
                                                                xs[:, a, k * 128:(k + 1) * 128], self.ident_bf[:]),
                         r=[("xs", a), "identb"], w=[pbn])
                P.dve(lambda e, a=a, pb=pb: e.tensor_tensor(
                    out=h[:, :, a * 128:(a + 1) * 128],
                    in0=pb[:].rearrange("p (k t) -> p k t", k=8),
                    in1=n1b.unsqueeze(2).to_broadcast([128, 8, 128]), op=ALU.mult),
                    r=[pbn, "n1"], w=[hname])
            P.dma("pool", lambda e: e.dma_start(out=self.HT[:, :, tsl], in_=h[:]),
                  r=[hname], w=[("HT", i)], key=f"hTst{i % 2}")

        def emit(chain):
            for eng, fn, r, w in chain:
                P.add(eng, fn, r, w)

        def zipchains(chains):
            n = max(len(c) for c in chains)
            for j in range(n):
                for c in chains:
                    if j < len(c):
                        P.add(*c[j])

        def dir_chain(i, hd, dr, bz, tset, tn, oh, ohn, qsb):
            t0, t1, b, E1, sm = (tset[(nm, dr)] for nm in ("t0", "t1", "b", "E1", "sm"))
            n0, n1_, nb, nE, ns = (tn((nm, dr)) for nm in ("t0", "t1", "b", "E1", "sm"))
            lbv = self.lb[:, dr, hd, l:l + 1]
            c = []
            c.append(("act", lambda e: e.activation(out=t0[:], in_=psF[bz][:], func=AF.Exp, scale=-1.0),
                      [("psF", bz)], [n0]))
            c.append(("act", lambda e: e.activation(out=t1[:], in_=t0[:], func=AF.Ln, scale=lbv, bias=1.0),
                      [n0, "lb"], [n1_]))
            c.append(("act", lambda e: e.activation(out=b[:], in_=t0[:], func=AF.Ln, bias=1.0), [n0], [nb]))
            c.append(("dve", lambda e: e.tensor_tensor(out=b[:], in0=t1[:], in1=b[:], op=ALU.subtract),
                      [n1_, nb], [nb]))
            c.append(("act", lambda e: e.activation(out=t0[:], in_=b[:], func=AF.Exp), [nb], [n0]))
            if dr == 0:
                c.append(("dve", lambda e: e.tensor_tensor_scan(
                    out=b[:], data0=self.rst[:], data1=b[:], initial=0.0, op0=ALU.mult, op1=ALU.add),
                    [nb, "rst"], [nb]))
                bv = b[:].rearrange("p (c j u) -> p c j u", c=8, j=2)[:, :, :, 31]
                mrow, Brow = 0, 1
            else:
                c.append(("dve", lambda e: e.tensor_tensor_scan(
                    out=b[:, ::-1], data0=self.rst[:], data1=b[:, ::-1], initial=0.0,
                    op0=ALU.mult, op1=ALU.add), [nb, "rst"], [nb]))
                bv = b[:].rearrange("p (c j u) -> p c j u", c=8, j=2)[:, :, :, 0]
                mrow, Brow = 1, 0
            c.append(("dve", lambda e: e.tensor_copy(out=sm[:, 0:2, :].rearrange("p j c -> p c j"), in_=bv),
                      [nb], [ns]))
            c.append(("dve", lambda e: e.tensor_tensor(out=sm[:, 2, :], in0=sm[:, Brow, :], in1=sm[:, mrow, :],
                                                       op=ALU.subtract), [ns], [ns]))
            for j, row in enumerate((Brow, mrow, 2)):
                c.append(("act", lambda e, row=row, j=j: e.activation(
                    out=self.scal[:, dr, hd, j, i * 8:(i + 1) * 8], in_=sm[:, row, :], func=AF.Exp),
                    [ns], [("scal", dr, hd, i)]))
            c.append(("dve", lambda e: e.tensor_tensor(
                out=b[:].rearrange("p (c u) -> p c u", c=8),
                in0=b[:].rearrange("p (c u) -> p c u", c=8),
                in1=sm[:, mrow, :].unsqueeze(2).to_broadcast([128, 8, CH]), op=ALU.subtract),
                [nb, ns], [nb]))
            c.append(("act", lambda e: e.activation(out=E1[:], in_=b[:], func=AF.Exp), [nb], [nE]))
            c.append(("act", lambda e: e.activation(out=b[:], in_=b[:], func=AF.Exp, scale=-1.0), [nb], [nb]))
            c.append(("dve", lambda e: e.tensor_tensor(out=oh[:, 2 * dr, :], in0=qsb[:], in1=E1[:], op=ALU.mult),
                      [tn("qsb"), nE], [ohn]))
            c.append(("dve", lambda e: e.scalar_tensor_tensor(
                out=oh[:, 2 * dr + 1, :], in0=t0[:], scalar=1.0, in1=b[:], op0=ALU.subtract, op1=ALU.mult),
                [n0, nb], [ohn]))
            return c

        def head_body(i, hd, g):
            tsl = slice(i * TS, (i + 1) * TS)
            h = hT[i % 2]
            hname = ("hT", i % 2)
            si = g % NSET
            tset = T[si]
            tn = lambda nm: (nm, si)
            oh = OH[g % 2]
            ohn = ("OH", g % 2)
            bzf, bzb = (0, 1) if g % 2 == 0 else (2, 3)
            bq, bi, bg = 4, 5, 6
            for c, bk in ((5 + hd, bzf), (10 + hd, bzb), (hd, bq), (15 + hd, bi), (20 + hd, bg)):
                for k in range(8):
                    P.pe(lambda e, bk=bk, k=k, c=c: e.matmul(psF[bk][:], win[:, k, c * 128:(c + 1) * 128],
                                                             h[:, k, :], start=(k == 0), stop=(k == 7)),
                         r=[hname, ("win", k)], w=[("psF", bk)])
            eq, qsb, eg, gsb = tset["eq"], tset["qsb"], tset["eg"], tset["gsb"]
            cB = []
            cB.append(("act", lambda e: e.activation(out=eq[:], in_=psF[bq][:], func=AF.Exp, scale=-1.0),
                       [("psF", bq)], [tn("eq")]))
            cB.append(("act", lambda e: e.copy(out=oh[:, 4, :], in_=psF[bi][:]), [("psF", bi)], [ohn]))
            cB.append(("act", lambda e: e.activation(out=eg[:], in_=psF[bg][:], func=AF.Exp, scale=-1.0),
                       [("psF", bg)], [tn("eg")]))
            for ee, nm in ((eq, "eq"), (eg, "eg")):
                cB.append(("act", lambda e, ee=ee: e.activation(out=ee[:], in_=ee[:], func=AF.Ln, bias=1.0),
                           [tn(nm)], [tn(nm)]))
                cB.append(("act", lambda e, ee=ee: e.activation(out=ee[:], in_=ee[:], func=AF.Exp, scale=-1.0),
                           [tn(nm)], [tn(nm)]))
            cB.append(("dve", lambda e: e.tensor_tensor(out=qsb[:], in0=psF[bq][:], in1=eq[:], op=ALU.mult),
                       [("psF", bq), tn("eq")], [tn("qsb")]))
            cB.append(("dve", lambda e: e.tensor_tensor(out=oh[:, 5, :], in0=psF[bg][:], in1=eg[:], op=ALU.mult),
                       [("psF", bg), tn("eg")], [ohn]))
            chains = [dir_chain(i, hd, 0, bzf, tset, tn, oh, ohn, qsb),
                      dir_chain(i, hd, 1, bzb, tset, tn, oh, ohn, qsb), cB]
            zipchains(chains)
            P.dma("pool", lambda e: e.dma_start(out=self.HG[:, hd, :, tsl], in_=oh[:]),
                  r=[ohn], w=[("HG", hd, i)], key=f"ohst{g % 2}")

        g = 0
        norm_tile(0)
        for i in range(self.nt):
            for hd in range(HG_H):
                if hd == 2 and i + 1 < self.nt:
                    norm_tile(i + 1)
                head_body(i, hd, g)
                g += 1


    def phase_p1b(self, l):
        nc, P, A = self.nc, self.P, self.ph
        P.barrier()
        A.reset()
        psF = self.psF
        wb = A.alloc("wb", [128, 8, WB], BF16)
        hT = [A.alloc(f"hTb{j}", [128, 8, TS], BF16) for j in range(2)]
        rC = [A.alloc(f"rC{j}", [128, TS], F32) for j in range(2)]
        rS = [A.alloc(f"rS{j}", [128, TS], F32) for j in range(2)]
        AOt = [A.alloc(f"AOt{j}", [128, 9, TS], BF16) for j in range(2)]
        t1 = [A.alloc(f"ta{j}", [128, TS], F32) for j in range(4)]
        t2 = [A.alloc(f"tb{j}", [128, TS], F32) for j in range(4)]
        for k in range(8):
            P.dma("pool", lambda e, k=k: e.dma_start(out=wb[:, k, :],
                                                     in_=self.w_in[l, k * 128:(k + 1) * 128, WA:WA + WB]),
                  w=[("wb", k)], key="wb")

        def load(i):
            j = i % 2
            tsl = slice(i * TS, (i + 1) * TS)
            P.dma("sp", lambda e: e.dma_start(out=hT[j][:], in_=self.HT[:, :, tsl]),
                  r=[("HT", i)], w=[("hTb", j)], key=f"hTb{j}")
            P.dma("sp", lambda e: e.dma_start(out=rC[j][:], in_=self.c_ropeC[:, tsl]), w=[("rC", j)], key=f"rC{j}")
            P.dma("sp", lambda e: e.dma_start(out=rS[j][:], in_=self.c_ropeS[:, tsl]), w=[("rS", j)], key=f"rS{j}")

        def mm(bank, col, h, hn):
            for k in range(8):
                P.pe(lambda e, k=k: e.matmul(psF[bank][:], wb[:, k, col * 128:(col + 1) * 128], h[:, k, :],
                                             start=(k == 0), stop=(k == 7)),
                     r=[hn, ("wb", k)], w=[("psF", bank)])

        def body(i, u):
            j = i % 2
            tsl = slice(i * TS, (i + 1) * TS)
            h, hn = hT[j], ("hTb", j)
            ao, aon = AOt[j], ("AOt", j)
            for g in range(3):
                bv = 6
                mm(bv, 6 + g, h, hn)
                P.act(lambda e, g=g: e.copy(out=ao[:, 6 + g, :], in_=psF[bv][:]), r=[("psF", bv)], w=[aon])
                for which, (c0, c1, slot) in enumerate(((g, 9 + g, g), (3 + g, 12 + g, 3 + g))):
                    b0, b1 = (0, 1) if (u[0] % 2 == 0) else (2, 3)
                    ti = u[0] % 4
                    u[0] += 1
                    mm(b0, c0, h, hn)
                    mm(b1, c1, h, hn)
                    ta, tb = t1[ti], t2[ti]
                    P.dve(lambda e, ta=ta, b0=b0: e.tensor_tensor(out=ta[:], in0=psF[b0][:], in1=rC[j][:], op=ALU.mult),
                          r=[("psF", b0), ("rC", j)], w=[("ta", ti)])
                    P.dve(lambda e, tb=tb, b1=b1: e.tensor_tensor(out=tb[:], in0=psF[b1][:], in1=rS[j][:], op=ALU.mult),
                          r=[("psF", b1), ("rS", j)], w=[("tb", ti)])
                    P.pool(lambda e, ta=ta, tb=tb, slot=slot: e.tensor_tensor(out=ao[:, slot, :], in0=ta[:], in1=tb[:],
                                                                              op=ALU.add),
                           r=[("ta", ti), ("tb", ti)], w=[aon])
            P.dma("pool", lambda e: e.dma_start(out=self.AT[:, :, tsl], in_=ao[:]), r=[aon], w=[("AT", i)],
                  key=f"aost{j}")

        u = [0]
        load(0)
        for i in range(self.nt):
            if i + 1 < self.nt:
                load(i + 1)
            body(i, u)

    def phase_p2(self, l):
        nc, P, A = self.nc, self.P, self.ph
        P.barrier()
        A.reset()
        psF = self.psF
        NTl = self.nt
        HGt = [[A.alloc(f"HGt{d}{j}", [128, HG_H, 3, TS], BF16) for j in range(2)] for d in range(2)]
        Ost = [[A.alloc(f"Ost{d}{j}", [128, HG_H, TS], BF16) for j in range(2)] for d in range(2)]
        S = [A.alloc(f"S{d}", [128, HG_H, 128], F32) for d in range(2)]
        Sbf = [A.alloc(f"Sbf{d}", [128, HG_H, 128], BF16) for d in range(2)]
        tmp = [A.alloc(f"tmpU{d}", [128, HG_H, 128], F32) for d in range(2)]
        ktok = [A.alloc(f"ktok{d}", [CH, HG_H, 128], BF16) for d in range(2)]
        vtok = [A.alloc(f"vtok{d}", [CH, HG_H, 128], BF16) for d in range(2)]
        Am = [A.alloc(f"Am{d}", [CH, HG_H, CH], BF16) for d in range(2)]
        for d in range(2):
            for row in range(2):
                P.dve(lambda e, d=d, row=row: e.tensor_tensor(
                    out=self.scal[:, d, :, row, 0:NTl * 8], in0=self.scal[:, d, :, row, 0:NTl * 8],
                    in1=self.flags[:, d, 0:NTl * 8].unsqueeze(1).to_broadcast([128, HG_H, NTl * 8]), op=ALU.mult),
                    r=[("scal", d, hd, i) for hd in range(HG_H) for i in range(NTl)] + ["flags"],
                    w=[("scalf", d)])
            P.dve(lambda e, d=d: e.memset(S[d][:], 0.0), w=[("S", d)])
            P.dve(lambda e, d=d: e.memset(Sbf[d][:], 0.0), w=[("Sbf", d)])

        def load(d, i, slot):
            tsl = slice(i * TS, (i + 1) * TS)
            t = HGt[d][slot]
            for jj, cidx in enumerate((2 * d, 2 * d + 1, 4)):
                P.dma("sp", lambda e, jj=jj, cidx=cidx: e.dma_start(out=t[:, :, jj, :], in_=self.HG[:, :, cidx, tsl]),
                      r=[("HG", hd, i) for hd in range(HG_H)], w=[("HGt", d, slot)], key=f"hg{d}{slot}")

        def views(d):
            T1 = psF[0 + d][0:CH, :].bitcast(BF16)[:, 0:640].rearrange("p (h x) -> p h x", h=HG_H)
            T2b = (psF[6] if d == 0 else None)
            return T1

        psT1 = [psF[0][0:CH, :].bitcast(BF16), psF[1][0:CH, :].bitcast(BF16)]
        psT2 = [self.psBt[0:CH, 0:512], self.psBt[0:CH, 512:1024]]
        seq = []
        for step in range(NTl * 8):
            for d in range(2):
                seq.append((step, d))
        for d in range(2):
            load(d, 0 if d == 0 else NTl - 1, 0)
        def step_body(step, d):
            ti = step // 8
            cc_ = step % 8
            i = ti if d == 0 else NTl - 1 - ti
            cc = cc_ if d == 0 else 7 - cc_
            c = i * 8 + cc
            slot = ti % 2
            if cc_ == 0 and ti + 1 < NTl:
                load(d, (ti + 1) if d == 0 else NTl - 2 - ti, (ti + 1) % 2)
            t = HGt[d][slot]
            tn_ = ("HGt", d, slot)
            cols = slice(cc * CH, (cc + 1) * CH)
            ost, ostn = Ost[d][slot], ("Ost", d, slot)
            bT = 0 + d
            bA = 2 + d
            bO = 4 + d
            kT = psF[bT][0:CH, :].bitcast(BF16)
            vT = psF[6][0:CH, :].bitcast(BF16) if d == 0 else self.psBt[0:CH, :]
            vTn = ("psF", 6) if d == 0 else ("psB", 0)
            for hd in range(HG_H):
                P.pe(lambda e, hd=hd, kT=kT: e.transpose(kT[:, hd * 128:(hd + 1) * 128], t[:, hd, 1, cols],
                                                         self.ident_bf[:]),
                     r=[tn_, "identb"], w=[("psF", bT)])
            for hd in range(HG_H):
                P.pe(lambda e, hd=hd, vT=vT: e.transpose(vT[:, hd * 128:(hd + 1) * 128], t[:, hd, 2, cols],
                                                         self.ident_bf[:]),
                     r=[tn_, "identb"], w=[vTn])
            P.act(lambda e, kT=kT, d=d: e.copy(out=ktok[d][:].rearrange("p h x -> p (h x)"), in_=kT[:, 0:640]),
                  r=[("psF", bT)], w=[("ktok", d)])
            P.act(lambda e, vT=vT, d=d: e.copy(out=vtok[d][:].rearrange("p h x -> p (h x)"), in_=vT[:, 0:640]),
                  r=[vTn], w=[("vtok", d)])
            for hd in range(HG_H):
                P.pe(lambda e, hd=hd: e.matmul(psF[bA][0:CH, hd * CH:(hd + 1) * CH], t[:, hd, 1, cols],
                                               t[:, hd, 0, cols], start=True, stop=True),
                     r=[tn_], w=[("psF", bA)])
            P.dve(lambda e, d=d: e.tensor_tensor(
                out=Am[d][:], in0=psF[bA][0:CH, 0:HG_H * CH].rearrange("p (h x) -> p h x", h=HG_H),
                in1=self.hmask[:, d, :].unsqueeze(1).to_broadcast([CH, HG_H, CH]), op=ALU.mult),
                r=[("psF", bA), "hmask"], w=[("Am", d)])
            for hd in range(HG_H):
                P.pe(lambda e, hd=hd, d=d: e.matmul(psF[bO][:, hd * CH:(hd + 1) * CH], vtok[d][:, hd, :],
                                                    Am[d][:, hd, :], start=True, stop=False),
                     r=[("vtok", d), ("Am", d)], w=[("psF", bO)])
                P.pe(lambda e, hd=hd, d=d: e.matmul(psF[bO][:, hd * CH:(hd + 1) * CH], Sbf[d][:, hd, :],
                                                    t[:, hd, 0, cols], start=False, stop=True),
                     r=[("Sbf", d), tn_], w=[("psF", bO)])
            P.act(lambda e: e.copy(out=ost[:, :, cols],
                                   in_=psF[bO][:, 0:HG_H * CH].rearrange("p (h x) -> p h x", h=HG_H)),
                  r=[("psF", bO)], w=[ostn])
            for hd in range(HG_H):
                bank, off = (bT, hd * 128) if hd < 4 else (bA, 0)
                P.pe(lambda e, hd=hd, d=d, bank=bank, off=off: e.matmul(
                    psF[bank][:, off:off + 128], ktok[d][:, hd, :], vtok[d][:, hd, :], start=True, stop=True),
                    r=[("ktok", d), ("vtok", d)], w=[("psF", bank)])
            c1b = self.scal[:, d, :, 2, c:c + 1]
            decb = self.scal[:, d, :, 0, c:c + 1]
            P.dve(lambda e, d=d, c1b=c1b: e.tensor_tensor(
                out=tmp[d][:, 0:4, :], in0=psF[bT][:].rearrange("p (h x) -> p h x", h=4),
                in1=c1b[:, 0:4, :].to_broadcast([128, 4, 128]), op=ALU.mult),
                r=[("psF", bT), ("scal", d, 0, i), ("scal", d, 1, i), ("scal", d, 2, i), ("scal", d, 3, i), ("scalf", d)],
                w=[("tmpU", d)])
            P.dve(lambda e, d=d, c1b=c1b: e.tensor_tensor(
                out=tmp[d][:, 4:5, :], in0=psF[bA][:, 0:128].unsqueeze(1),
                in1=c1b[:, 4:5, :].to_broadcast([128, 1, 128]), op=ALU.mult),
                r=[("psF", bA), ("scal", d, 4, i), ("scalf", d)], w=[("tmpU", d)])
            P.dve(lambda e, d=d, decb=decb: e.tensor_tensor(
                out=S[d][:], in0=S[d][:], in1=decb.to_broadcast([128, HG_H, 128]), op=ALU.mult),
                r=[("S", d), ("scalf", d)] + [("scal", d, hd, i) for hd in range(HG_H)], w=[("S", d)])
            P.dve(lambda e, d=d: e.tensor_tensor(out=S[d][:], in0=S[d][:], in1=tmp[d][:], op=ALU.subtract),
                  r=[("S", d), ("tmpU", d)], w=[("S", d)])
            cn = c + 1 if d == 0 else c - 1
            if 0 <= cn < NTl * 8:
                emb = self.scal[:, d, :, 1, cn:cn + 1]
                inext = cn // 8
                P.pool(lambda e, d=d, emb=emb: e.tensor_tensor(
                    out=Sbf[d][:], in0=S[d][:], in1=emb.to_broadcast([128, HG_H, 128]), op=ALU.mult),
                    r=[("S", d), ("scalf", d)] + [("scal", d, hd, inext) for hd in range(HG_H)], w=[("Sbf", d)])
            if cc_ == 7:
                tsl = slice(i * TS, (i + 1) * TS)
                P.dma("pool", lambda e, ost=ost, d=d, tsl=tsl: e.dma_start(out=self.OF[:, d, :, tsl], in_=ost[:]),
                      r=[ostn], w=[("OF", d, i)], key=f"ofst{d}{slot}")

        for step, d in seq:
            step_body(step, d)

    def phase_p3(self, l):
        nc, P, A = self.nc, self.P, self.ph
        P.barrier()
        A.reset()
        psF = self.psF
        NS = max(1, self.nt // 4)
        SP = 2048
        qs = A.alloc("qs", [128, 3, SP], BF16)
        kv = A.alloc("kv", [128, 6, 2 * SP], BF16)
        UL = A.alloc("UL", [128, 2, 3, SP], F32)
        Pt = [A.alloc(f"Pt{j}", [128, 512], BF16) for j in range(2)]
        vpad = [A.alloc(f"vpad{j}", [128, 2, 128], BF16) for j in range(3)]
        onesp = A.alloc("onesp", [128, 2, 128], BF16)
        AOo = A.alloc("AOo", [128, 3, SP], BF16)
        Rc = A.alloc("Rc", [128, SP], F32)
        for j in range(3):
            P.dve(lambda e, j=j: e.memset(vpad[j][:], 0.0), w=[("vpad", j)])
        P.dve(lambda e: e.memset(onesp[:], 0.0), w=["onesp"])
        P.dve(lambda e: e.memset(onesp[:, 0, 0:64], 1.0), w=["onesp"])
        P.dve(lambda e: e.memset(onesp[:, 1, 64:128], 1.0), w=["onesp"])
        ntok = self.nt * TS
        cnt = {"s": 0, "v": 0}

        def span(s):
            t0 = s * SP
            lo, hi = max(0, t0 - 1024), min(ntok, t0 + SP + 1024)
            off = lo - (t0 - 1024)
            tiles_q = [("AT", i) for i in range(t0 // TS, (t0 + SP) // TS)]
            tiles_kv = [("AT", i) for i in range(lo // TS, hi // TS)]
            P.dma("sp", lambda e: e.dma_start(out=qs[:], in_=self.AT[:, 0:3, t0:t0 + SP]), r=tiles_q, w=["qs"], key="qsld")
            if off > 0:
                P.dve(lambda e: e.memset(kv[:, :, 0:off], 0.0), w=["kv"])
            if off + (hi - lo) < 2 * SP:
                P.dve(lambda e: e.memset(kv[:, :, off + (hi - lo):2 * SP], 0.0), w=["kv"])
            P.dma("sp", lambda e: e.dma_start(out=kv[:, :, off:off + (hi - lo)], in_=self.AT[:, 3:9, lo:hi]),
                  r=tiles_kv, w=["kv"], key="kvld")
            def kcols(r, rho, j):
                st_ = 1024 + (128 * j - 64) * r + rho
                return slice(st_, st_ + 127 * r + 1, r)

            def mk_v(g, r, rho, j):
                vi = cnt["v"] % 3
                cnt["v"] += 1
                reg = slice((vi % 4) * 128, (vi % 4) * 128 + 128)
                kc = kcols(r, rho, j)
                P.pe(lambda e: e.transpose(self.psBt[:, reg], kv[:, 3 + g, kc], self.ident_bf[:]),
                     r=["kv", "identb"], w=[("psB", 0)])
                P.act(lambda e: e.copy(
                    out=vpad[vi][:].rearrange("p h x -> p (h x)").rearrange("p (a b) -> p a b", b=64)[:, 0:4:3, :],
                    in_=self.psBt[:, reg].rearrange("p (a b) -> p a b", b=64)),
                    r=[("psB", 0)], w=[("vpad", vi)])
                return vi

            def block(g, r, rho, bi, vt, kinds):
                si = cnt["s"] % 2
                cnt["s"] += 1
                bS, bO = si, 2 + si
                qc = slice(128 * bi * r + rho, 128 * bi * r + rho + 127 * r + 1, r)
                for hh in range(2):
                    for tt in range(2):
                        c0 = (hh * 2 + tt) * 128
                        kc = kcols(r, rho, bi + tt)
                        P.pe(lambda e, hh=hh, c0=c0, kc=kc: e.matmul(
                            psF[bS][:, c0:c0 + 128], kv[hh * 64:(hh + 1) * 64, g, kc],
                            qs[hh * 64:(hh + 1) * 64, g, qc], start=True, stop=False),
                            r=["kv", "qs"], w=[("psF", bS)])
                        P.pe(lambda e, c0=c0, tt=tt, kd=kinds[tt]: e.matmul(
                            psF[bS][:, c0:c0 + 128], self.ident_bf[:], self.amask[:, kd, tt, :],
                            start=False, stop=True),
                            r=["identb", "amask"], w=[("psF", bS)])
                pt = Pt[si]
                P.act(lambda e: e.activation(out=pt[:], in_=psF[bS][:], func=AF.Exp, scale=SCALE_Q),
                      r=[("psF", bS)], w=[("Pt", si)])
                n = 0
                for hh in range(2):
                    for tt in range(2):
                        c0 = (hh * 2 + tt) * 128
                        P.pe(lambda e, hh=hh, tt=tt, c0=c0, n=n: e.matmul(
                            psF[bO][:, 0:128], vpad[vt[tt]][:, hh, :], pt[:, c0:c0 + 128],
                            start=(n == 0), stop=(n == 3)),
                            r=[("vpad", vt[tt]), ("Pt", si)], w=[("psF", bO)])
                        n += 1
                n = 0
                for hh in range(2):
                    for tt in range(2):
                        c0 = (hh * 2 + tt) * 128
                        P.pe(lambda e, hh=hh, c0=c0, n=n: e.matmul(
                            psF[bO][:, 128:256], onesp[:, hh, :], pt[:, c0:c0 + 128],
                            start=(n == 0), stop=(n == 3)),
                            r=["onesp", ("Pt", si)], w=[("psF", bO)])
                        n += 1
                P.act(lambda e: e.copy(out=UL[:, :, g, qc],
                                       in_=psF[bO][:, 0:256].rearrange("p (a b) -> p a b", a=2)),
                      r=[("psF", bO)], w=["UL"])

            for g, r in enumerate((1, 4, 16)):
                nb = (SP // r) // 128
                for rho in range(r):
                    vprev = mk_v(g, r, rho, 0)
                    for bi in range(nb):
                        vnext = mk_v(g, r, rho, bi + 1)
                        kinds = [0, 0]
                        if bi == 0:
                            kinds[0] = 1 if s == 0 else 2
                        if bi == nb - 1:
                            kinds[1] = 1 if s == NS - 1 else 2
                        block(g, r, rho, bi, (vprev, vnext), kinds)
                        vprev = vnext
            P.dve(lambda e: e.tensor_tensor(out=Rc[:], in0=UL[:, 1, 0, :], in1=UL[:, 1, 1, :], op=ALU.add),
                  r=["UL"], w=["Rc"])
            P.dve(lambda e: e.tensor_tensor(out=Rc[:], in0=Rc[:], in1=UL[:, 1, 2, :], op=ALU.add),
                  r=["UL", "Rc"], w=["Rc"])
            P.dve(lambda e: e.reciprocal(out=Rc[:], in_=Rc[:]), r=["Rc"], w=["Rc"])
            for g in range(3):
                P.dve(lambda e, g=g: e.tensor_tensor(out=AOo[:, g, :], in0=UL[:, 0, g, :], in1=Rc[:], op=ALU.mult),
                      r=["UL", "Rc"], w=["AOo"])
            P.dma("pool", lambda e: e.dma_start(out=self.AO[:, :, t0:t0 + SP], in_=AOo[:]), r=["AOo"],
                  w=[("AO", i) for i in range(t0 // TS, (t0 + SP) // TS)], key="aoo")

        for s in range(NS):
            span(s)

    def phase_p4(self, l):
        nc, P, A = self.nc, self.P, self.ph
        P.barrier()
        A.reset()
        psF = self.psF
        wo = A.alloc("wo", [128, 8, D], BF16)
        wr = A.alloc("wr", [128, 8, NE], BF16)
        OFt = [A.alloc(f"OFt{j}", [128, 2, HG_H, TS], BF16) for j in range(2)]
        sgt = [A.alloc(f"sgt{j}", [128, HG_H, TS], BF16) for j in range(2)]
        att = [A.alloc(f"att{j}", [128, 3, TS], BF16) for j in range(2)]
        xt = [A.alloc(f"xt4{j}", [128, 4, D], F32) for j in range(2)]
        ob = A.alloc("ob", [128, HG_H, TS], BF16)
        sq = A.alloc("sq", [128, HG_H, TS], BF16)
        lnt = [A.alloc(f"lnt{j}", [128, TS], F32) for j in range(2)]
        hg = A.alloc("hg", [128, HG_H, TS], F32)
        mix = A.alloc("mix", [128, HG_H, TS], BF16)
        junk = A.alloc("junk4", [128, D], BF16)
        ss = A.alloc("ss4", [128, 4], F32)
        rstd = A.alloc("rstd4", [128, 4], F32)
        xs = A.alloc("xs4", [128, 4, D], BF16)
        xnT = [A.alloc(f"xnT{j}", [128, 8, TS], BF16) for j in range(2)]
        mx = A.alloc("mx", [128, 4], F32)
        sm = A.alloc("smx", [128, 4], F32)
        xk = A.alloc("xk", [128, 4, D], BF16)
        n2row = A.alloc("n2row", [128, D], F32)
        P.dma("sp", lambda e: e.dma_start(out=n2row[:], in_=self.c_n2row[l:l + 1, :].to_broadcast([128, D])),
              w=["n2row"], key="n2row")
        ex = A.alloc("ex", [128, 4, NE], F32)
        for k in range(8):
            P.dma("pool", lambda e, k=k: e.dma_start(out=wo[:, k, :], in_=self.w_out[l, k * 128:(k + 1) * 128, :]),
                  w=[("wo", k)], key="wo")
        P.dma("pool", lambda e: e.dma_start(out=wr[:], in_=self.w_router[l].rearrange("(k p) n -> p k n", p=128)),
              w=["wr"], key="wr")
        for k in range(HG_H):
            P.dve(lambda e, k=k: e.tensor_scalar(out=wo[:, k, :], in0=wo[:, k, :], scalar1=self.hgw[:, l, k:k + 1],
                                                 scalar2=None, op0=ALU.mult),
                  r=[("wo", k), "hgw"], w=[("wo", k)])
        n2b = self.n2[:, l, :]

        def load(i):
            j = i % 2
            tsl = slice(i * TS, (i + 1) * TS)
            for d in range(2):
                P.dma("sp", lambda e, d=d: e.dma_start(out=OFt[j][:, d, :, :], in_=self.OF[:, d, :, tsl]),
                      r=[("OF", d, i)], w=[("OFt", j)], key=f"oft{j}")
            P.dma("sp", lambda e: e.dma_start(out=sgt[j][:], in_=self.HG[:, :, 5, tsl]),
                  r=[("HG", hd, i) for hd in range(HG_H)], w=[("sgt", j)], key=f"sgt{j}")
            P.dma("sp", lambda e: e.dma_start(out=att[j][:], in_=self.AO[:, :, tsl]), r=[("AO", i)], w=[("att", j)],
                  key=f"att{j}")
            P.dma("sp", lambda e: e.dma_start(
                out=xt[j][:], in_=self.XA[i * TS:(i + 1) * TS, :].rearrange("(a p) f -> p a f", p=128)),
                r=[("XA", i)], w=[("xt4", j)], key=f"xt4{j}")

        def body(i):
            j = i % 2
            tsl = slice(i * TS, (i + 1) * TS)
            x = xt[j]
            xn_ = ("xt4", j)
            P.dve(lambda e: e.tensor_tensor(out=ob[:], in0=OFt[j][:, 0, :, :], in1=OFt[j][:, 1, :, :], op=ALU.add),
                  r=[("OFt", j)], w=["ob"])
            P.act(lambda e: e.activation(out=sq[:], in_=ob[:], func=AF.Square), r=["ob"], w=["sq"])
            for hd in range(HG_H):
                b = hd % 2
                P.pe(lambda e, hd=hd, b=b: e.matmul(psF[b][:], self.ones_bf[:], sq[:, hd, :], start=True, stop=True),
                     r=["onesb", "sq"], w=[("psF", b)])
                lt = lnt[b]
                P.act(lambda e, b=b, lt=lt: e.activation(out=lt[:], in_=psF[b][:], func=AF.Ln, scale=1.0 / 128,
                                                         bias=self.epsb[:]),
                      r=[("psF", b), "epsb"], w=[("lnt", b)])
                P.act(lambda e, lt=lt: e.activation(out=lt[:], in_=lt[:], func=AF.Exp, scale=-0.5),
                      r=[("lnt", b)], w=[("lnt", b)])
                P.dve(lambda e, hd=hd, lt=lt: e.tensor_tensor(out=hg[:, hd, :], in0=ob[:, hd, :], in1=lt[:],
                                                              op=ALU.mult),
                      r=["ob", ("lnt", b)], w=[("hg", hd)])
                P.dve(lambda e, hd=hd: e.tensor_tensor(out=mix[:, hd, :], in0=hg[:, hd, :], in1=sgt[j][:, hd, :],
                                                       op=ALU.mult),
                      r=[("hg", hd), ("sgt", j)], w=[("mix", hd)])
            for a in range(4):
                for n in range(2):
                    b = 2 + (a * 2 + n) % 4
                    for k in range(8):
                        if k < HG_H:
                            lh, ln_ = mix[:, k, a * 128:(a + 1) * 128], ("mix", k)
                        else:
                            lh, ln_ = att[j][:, k - HG_H, a * 128:(a + 1) * 128], ("att", j)
                        P.pe(lambda e, b=b, k=k, n=n, lh=lh: e.matmul(psF[b][:], lh, wo[:, k, n * 512:(n + 1) * 512],
                                                                      start=(k == 0), stop=(k == 7)),
                             r=[ln_, ("wo", k)], w=[("psF", b)])
                    P.dve(lambda e, a=a, n=n, b=b: e.tensor_tensor(
                        out=x[:, a, n * 512:(n + 1) * 512], in0=x[:, a, n * 512:(n + 1) * 512], in1=psF[b][:],
                        op=ALU.add), r=[xn_, ("psF", b)], w=[xn_])
            P.dma("pool", lambda e: e.dma_start(
                out=self.XM[i * TS:(i + 1) * TS, :].rearrange("(a p) f -> p a f", p=128), in_=x[:]),
                r=[xn_], w=[("XM", i)], key=f"xmst{j}")
            P.dve(lambda e: e.memset(ss[:], 0.0), w=["ss4"])
            for a in range(4):
                P.act(lambda e, a=a: e.activation(out=junk[:], in_=x[:, a, :], func=AF.Square,
                                                  accum_out=ss[:, a:a + 1]), r=[xn_, "ss4"], w=["junk4", "ss4"])
            P.act(lambda e: e.activation(out=rstd[:], in_=ss[:], func=AF.Ln, scale=1.0 / D, bias=self.epsb[:]),
                  r=["ss4", "epsb"], w=["rstd4"])
            P.act(lambda e: e.activation(out=rstd[:], in_=rstd[:], func=AF.Exp, scale=-0.5), r=["rstd4"], w=["rstd4"])
            for a in range(4):
                P.dve(lambda e, a=a: e.tensor_scalar(out=xs[:, a, :], in0=x[:, a, :], scalar1=rstd[:, a:a + 1],
                                                     scalar2=None, op0=ALU.mult), r=[xn_, "rstd4"], w=[("xs4", a)])
            for a in range(4):
                P.dve(lambda e, a=a: e.tensor_tensor(out=xk[:, a, :], in0=xs[:, a, :], in1=n2row[:], op=ALU.mult),
                      r=[("xs4", a), "n2row"], w=["xk"])
            P.dma("pool", lambda e: e.dma_start(
                out=self.XNK[i * TS:(i + 1) * TS, :].rearrange("(a p) f -> p a f", p=128), in_=xk[:]),
                r=["xk"], w=[("XNK", i)], key="xkst")
            xT = xnT[j]
            for a in range(4):
                for k in range(8):
                    P.pe(lambda e, a=a, k=k: e.transpose(self.psBt[:, k * 128:(k + 1) * 128],
                                                         xs[:, a, k * 128:(k + 1) * 128], self.ident_bf[:]),
                         r=[("xs4", a), "identb"], w=[("psB", 0)])
                P.dve(lambda e, a=a: e.tensor_tensor(
                    out=xT[:, :, a * 128:(a + 1) * 128], in0=self.psBt[:].rearrange("p (k t) -> p k t", k=8),
                    in1=n2b.unsqueeze(2).to_broadcast([128, 8, 128]), op=ALU.mult),
                    r=[("psB", 0), "n2"], w=[("xnT", j)])
            P.dma("pool", lambda e: e.dma_start(out=self.XN[:, :, tsl], in_=xT[:]), r=[("xnT", j)], w=[("XN", i)],
                  key=f"xnst{j}")
            for a in range(4):
                for k in range(8):
                    P.pe(lambda e, a=a, k=k: e.matmul(psF[6][:, a * NE:(a + 1) * NE], xT[:, k, a * 128:(a + 1) * 128],
                                                      wr[:, k, :], start=(k == 0), stop=(k == 7)),
                         r=[("xnT", j), "wr"], w=[("psF", 6)])
            lg = psF[6][:, 0:4 * NE].rearrange("p (a n) -> p a n", a=4)
            P.dve(lambda e: e.tensor_reduce(out=mx[:], in_=lg, axis=AX.X, op=ALU.max), r=[("psF", 6)], w=["mx"])
            P.dve(lambda e: e.tensor_scalar(out=mx[:], in0=mx[:], scalar1=-1.0, scalar2=None, op0=ALU.mult),
                  r=["mx"], w=["mx"])
            P.dve(lambda e: e.memset(sm[:], 0.0), w=["smx"])
            for a in range(4):
                P.act(lambda e, a=a: e.activation(out=ex[:, a, :], in_=psF[6][:, a * NE:(a + 1) * NE], func=AF.Exp,
                                                  bias=mx[:, a:a + 1], accum_out=sm[:, a:a + 1]),
                      r=[("psF", 6), "mx", "smx"], w=["ex", "smx"])
            P.dve(lambda e: e.reciprocal(out=sm[:], in_=sm[:]), r=["smx"], w=["smx"])
            P.dve(lambda e: e.tensor_tensor(out=self.aff[:, i * 4:(i + 1) * 4, :], in0=ex[:],
                                            in1=sm[:].unsqueeze(2).to_broadcast([128, 4, NE]), op=ALU.mult),
                  r=["ex", "smx"], w=[("aff", i)])

        load(0)
        for i in range(self.nt):
            if i + 1 < self.nt:
                load(i + 1)
            body(i)

    def phase_p67(self, l, last):
        nc, P, A = self.nc, self.P, self.ph
        P.barrier()
        A.reset()
        psF = self.psF
        NBLK = self.nt * 4
        NTK = NBLK * 128
        CAPG = 2 * (4 * NTK) // NE
        Gt = A.alloc("Gt", [128, NBLK, NE], F32)
        mark = A.cur
        affT = A.alloc("affT", [NE, NTK], F32)
        G = A.alloc("G", [64, NTK], F32)
        sgn = A.alloc("sgn", [64, NTK], BF16)
        small = A.alloc("small", [64, 16], F32)
        gsum = A.alloc("gsum", [64, 64], F32)
        onesf = A.alloc("onesf", [64, 128], F32)
        Dm = A.alloc("Dm", [64, NE], F32)
        thr = A.alloc("thr", [128, NE], F32)
        for b4 in range(NBLK // 4):
            bank = b4 % 2
            for q in range(4):
                blk = b4 * 4 + q
                P.pe(lambda e, blk=blk, q=q, bank=bank: e.matmul(psF[bank][0:NE, q * 128:(q + 1) * 128],
                                                                 self.aff[:, blk, :], self.ident_f[:],
                                                                 start=True, stop=True),
                     r=[("aff", blk // 4), "identf"], w=[("psF", bank)])
            P.act(lambda e, b4=b4, bank=bank: e.copy(out=affT[:, b4 * 512:(b4 + 1) * 512], in_=psF[bank][0:NE, :]),
                  r=[("psF", bank)], w=["affT"])
        P.dma("sp", lambda e: e.dma_start(out=self.AFL[:, 0:NTK], in_=affT[:]), r=["affT"], w=["AFL"], key="afl")
        P.dma("pool", lambda e: e.collective_compute("AllGather", ALU.bypass, replica_groups=[[0, 1, 2, 3], [4, 5, 6, 7]],
                                                     ins=[self.AFL.opt()], outs=[self.AFG.opt()]),
              r=["AFL"], w=["AFG"], key="cc", inc=1)
        P.dma("sp", lambda e: e.dma_start(out=G[:], in_=self.AFG[:, 0:NTK]), r=["AFG"], w=["G"], key="gld")
        P.dma("sp", lambda e: e.dma_start(out=gsum[:], in_=self.c_gsum[:, :]), w=["gsum"], key="gsum")
        P.dma("sp", lambda e: e.dma_start(out=Dm[:], in_=self.c_dsel[:, :]), w=["Dm0"], key="dsel")
        P.dve(lambda e: e.memset(onesf[:], 1.0), w=["onesf"])
        P.dve(lambda e: e.memset(small[:], 0.0), w=["small"])
        P.dve(lambda e: e.memset(small[:, 1:2], 1.0), r=["small"], w=["small"])
        P.dve(lambda e: e.memset(small[:, 2:3], -0.5), r=["small"], w=["small"])
        LO, HI, NM, CNT, TOT, MM, DD, D2, SS = (slice(j, j + 1) for j in range(9))
        target = float(2 * CAPG - 4 * NTK)
        for it in range(26):
            P.dve(lambda e: e.memset(small[:, CNT], 0.0), r=["small"], w=["small"])
            P.act(lambda e: e.activation(out=sgn[:], in_=G[:], func=AF.Sign, bias=small[:, NM],
                                         accum_out=small[:, CNT]), r=["G", "small"], w=["sgn", "small"])
            P.pe(lambda e: e.matmul(psF[2][0:64, 0:1], gsum[:], small[:, CNT], start=True, stop=True),
                 r=["gsum", "small"], w=[("psF", 2)])
            P.dve(lambda e: e.tensor_scalar(out=small[:, MM], in0=psF[2][0:64, 0:1], scalar1=target, scalar2=None,
                                            op0=ALU.is_ge), r=[("psF", 2)], w=["small"])
            P.dve(lambda e: e.scalar_tensor_tensor(out=small[:, DD], in0=small[:, NM], scalar=-1.0, in1=small[:, LO],
                                                   op0=ALU.mult, op1=ALU.subtract), r=["small"], w=["small"])
            P.dve(lambda e: e.tensor_tensor(out=small[:, D2], in0=small[:, HI], in1=small[:, NM], op=ALU.add),
                  r=["small"], w=["small"])
            P.dve(lambda e: e.scalar_tensor_tensor(out=small[:, LO], in0=small[:, DD], scalar=small[:, MM],
                                                   in1=small[:, LO], op0=ALU.mult, op1=ALU.add),
                  r=["small"], w=["small"])
            P.dve(lambda e: e.scalar_tensor_tensor(out=small[:, HI], in0=small[:, D2], scalar=small[:, MM],
                                                   in1=small[:, NM], op0=ALU.mult, op1=ALU.subtract),
                  r=["small"], w=["small"])
            P.dve(lambda e: e.tensor_tensor(out=small[:, SS], in0=small[:, LO], in1=small[:, HI], op=ALU.add),
                  r=["small"], w=["small"])
            P.dve(lambda e: e.tensor_scalar(out=small[:, NM], in0=small[:, SS], scalar1=-0.5, scalar2=None,
                                            op0=ALU.mult), r=["small"], w=["small"])
        P.dve(lambda e: e.tensor_scalar(out=Dm[:], in0=Dm[:], scalar1=small[:, LO], scalar2=None, op0=ALU.mult),
              r=["Dm0", "small"], w=["Dm"])
        P.pe(lambda e: e.matmul(psF[3][:, 0:NE], onesf[:], Dm[:], start=True, stop=True), r=["onesf", "Dm"],
             w=[("psF", 3)])
        P.act(lambda e: e.copy(out=thr[:], in_=psF[3][:, 0:NE]), r=[("psF", 3)], w=["thr"])
        affv = self.aff[:, 0:NBLK, :]
        P.dve(lambda e: e.tensor_tensor(out=Gt[:], in0=affv, in1=thr[:].unsqueeze(1).to_broadcast([128, NBLK, NE]),
                                        op=ALU.is_ge), r=[("aff", i) for i in range(self.nt)] + ["thr"], w=["Gt"])
        P.dve(lambda e: e.tensor_tensor(out=Gt[:], in0=Gt[:], in1=affv, op=ALU.mult),
              r=[("aff", i) for i in range(self.nt)] + ["Gt"], w=["Gt"])

        if "GT" in self.debug:
            P.dma("sp", lambda e: e.dma_start(out=self.GTd[:, :, :], in_=Gt[:]), r=["Gt"], w=["GTd"], key="gtd")
            P.dma("sp", lambda e: e.dma_start(out=self.THd[:, :], in_=small[:]), r=["small"], w=["THd"], key="thd")
        P.barrier()
        A.cur = mark
        NEX = self.nexp
        STT = 1024
        NST = NTK // STT
        W = [[A.alloc(f"W{nm}{j}", [128, 8, D], BF16) for j in range(2)] for nm in range(3)]
        xnT = A.alloc("xnT7", [128, 8, STT if not self.sparse else 8], BF16)
        acc = A.alloc("acc", [128, 8, D], F32)
        hT = A.alloc("hT7", [128, 8, TS if not self.sparse else 128], BF16)
        sg = [A.alloc(f"sg7{j}", [128, TS if not self.sparse else 8], BF16) for j in range(2)]
        xm1 = A.alloc("xm70", [128, D], F32)
        xm = [xm1, xm1]
        junk = hT[:].rearrange("p k t -> p (k t)")[:, 0:D]
        ss = A.alloc("ss7", [128, 8], F32)
        rstd = A.alloc("rstd7", [128, 8], F32)
        fnw = A.alloc("fnw", [128, D], F32)
        if last:
            P.dma("sp", lambda e: e.dma_start(out=fnw[:], in_=self.c_fnw.to_broadcast([128, D])), w=["fnw"], key="fnw")
        wsrc = (self.w_gate, self.w_up, self.w_down)
        cnt = {"w": 0, "g": 0, "d": 0}

        def loadw(e_):
            j = cnt["w"] % 2
            cnt["w"] += 1
            for nm in range(3):
                for kh in range(2):
                    P.dma("pool", lambda e, nm=nm, kh=kh: e.dma_start(
                        out=W[nm][j][:, kh * 4:(kh + 1) * 4, :],
                        in_=wsrc[nm][l, e_, kh * 512:(kh + 1) * 512, :].rearrange("(k p) f -> p k f", p=128)),
                        w=[("W", nm, j)], key=f"w{nm}{j}")
            return j

        def expert(st, e_, j):
            Wg, Wu, Wd = W[0][j], W[1][j], W[2][j]
            for tl in range(STT // TS):
                for m in range(8):
                    gi = cnt["g"] % 2
                    cnt["g"] += 1
                    bG, bU = gi, 2 + gi
                    for k in range(8):
                        P.pe(lambda e, k=k, m=m, bG=bG, tl=tl: e.matmul(psF[bG][:], Wg[:, k, m * 128:(m + 1) * 128],
                                                                 xnT[:, k, tl * TS:(tl + 1) * TS],
                                                                 start=(k == 0), stop=(k == 7)),
                             r=[("W", 0, j), "xnT7"], w=[("psF", bG)])
                    for k in range(8):
                        P.pe(lambda e, k=k, m=m, bU=bU, tl=tl: e.matmul(psF[bU][:], Wu[:, k, m * 128:(m + 1) * 128],
                                                                 xnT[:, k, tl * TS:(tl + 1) * TS],
                                                                 start=(k == 0), stop=(k == 7)),
                             r=[("W", 1, j), "xnT7"], w=[("psF", bU)])
                    P.act(lambda e, gi=gi, bG=bG: e.activation(out=sg[gi][:], in_=psF[bG][:], func=AF.Silu),
                          r=[("psF", bG)], w=[("sg7", gi)])
                    P.dve(lambda e, gi=gi, bU=bU, m=m: e.tensor_tensor(out=hT[:, m, :], in0=psF[bU][:], in1=sg[gi][:],
                                                                       op=ALU.mult),
                          r=[("psF", bU), ("sg7", gi)], w=[("hT7", m)])
                for sub in range(4):
                    s8 = tl * 4 + sub
                    blk = st * 8 + s8
                    gate = Gt[:, blk, e_:e_ + 1]
                    for n in range(2):
                        bD = 4 + cnt["d"] % 3
                        cnt["d"] += 1
                        for k in range(8):
                            P.pe(lambda e, k=k, n=n, bD=bD, sub=sub: e.matmul(
                                psF[bD][:], hT[:, k, sub * 128:(sub + 1) * 128], Wd[:, k, n * 512:(n + 1) * 512],
                                start=(k == 0), stop=(k == 7)),
                                r=[("hT7", k), ("W", 2, j)], w=[("psF", bD)])
                        asl = acc[:, s8, n * 512:(n + 1) * 512]
                        if e_ == 0:
                            P.dve(lambda e, bD=bD, asl=asl, gate=gate: e.tensor_scalar(
                                out=asl, in0=psF[bD][:], scalar1=gate, scalar2=None, op0=ALU.mult),
                                r=[("psF", bD), "Gt"], w=[("acc", s8)])
                        else:
                            P.dve(lambda e, bD=bD, asl=asl, gate=gate: e.scalar_tensor_tensor(
                                out=asl, in0=psF[bD][:], scalar=gate, in1=asl, op0=ALU.mult, op1=ALU.add),
                                r=[("psF", bD), "Gt", ("acc", s8)], w=[("acc", s8)])

        CAPS = 128
        tri = A.alloc("tri", [128, 128], BF16)
        onesb2 = self.ones_bf
        iota = A.alloc("iota", [128, 4, CAPS], F32)
        xtok = A.alloc("xtok", [128, 8, D], BF16)
        m01 = A.alloc("m01", [128, 8, NE], BF16)
        posm = A.alloc("posm", [128, 8, NE], F32)
        offs = A.alloc("offs", [128, 8, NE], F32)
        S_ = [A.alloc(f"S_{q}", [128, 4, CAPS], BF16) for q in range(2)]
        ST = [A.alloc(f"ST{q}", [128, 4, 128], BF16) for q in range(2)]
        xeT = A.alloc("xeT", [128, 8, CAPS], BF16)
        hTs = A.alloc("hTs", [128, 8, CAPS], BF16)
        sgs = A.alloc("sgs", [128, 4, CAPS], BF16)
        yeb1 = A.alloc("yeb0", [128, D], BF16)
        yeb = [yeb1, yeb1]
        P.dma("pool", lambda e: e.dma_start(out=tri[:], in_=self.c_tri[:, :]), w=["tri"], key="tri")
        P.dma("sp", lambda e: e.dma_start(out=iota[:], in_=self.c_iota[:, :, :]), w=["iota"], key="iota")
        scnt = {"s": 0, "y": 0, "d": 0}

        def positions(st):
            gts = Gt[:, st * 8:(st + 1) * 8, :]
            P.dve(lambda e: e.tensor_scalar(out=m01[:], in0=gts, scalar1=0.0, scalar2=None, op0=ALU.is_gt),
                  r=["Gt"], w=["m01"])
            mflat = m01[:].rearrange("p b n -> p (b n)")
            P.pe(lambda e: e.matmul(psF[6][:, 0:128], tri[:], mflat, start=True, stop=True), r=["tri", "m01"],
                 w=[("psF", 6)])
            P.pe(lambda e: e.matmul(psF[6][:, 128:256], onesb2[:], mflat, start=True, stop=True), r=["onesb", "m01"],
                 w=[("psF", 6)])
            within = psF[6][:, 0:128].rearrange("p (b n) -> p b n", b=8)
            cntb = psF[6][:, 128:256].rearrange("p (b n) -> p b n", b=8)
            P.dve(lambda e: e.memset(offs[:], 0.0), w=["offs"])
            for tl in range(2):
                for q in range(1, 4):
                    b = tl * 4 + q
                    P.dve(lambda e, b=b: e.tensor_tensor(out=offs[:, b, :], in0=offs[:, b - 1, :], in1=cntb[:, b - 1, :],
                                                         op=ALU.add), r=["offs", ("psF", 6)], w=["offs"])
            P.dve(lambda e: e.tensor_tensor(out=offs[:], in0=offs[:], in1=within, op=ALU.add),
                  r=["offs", ("psF", 6)], w=["offs"])
            P.dve(lambda e: e.scalar_tensor_tensor(out=posm[:], in0=offs[:], scalar=1.0, in1=m01[:], op0=ALU.add,
                                                   op1=ALU.mult), r=["offs", "m01"], w=["posm"])
            P.dve(lambda e: e.tensor_scalar_add(out=posm[:], in0=posm[:], scalar1=-1.0), r=["posm"], w=["posm"])

        def expert_sparse(st, e_, j):
            Wg, Wu, Wd = W[0][j], W[1][j], W[2][j]
            for tl in range(2):
                q = scnt["s"] % 2
                scnt["s"] += 1
                Sq, STq = S_[q], ST[q]
                P.dve(lambda e, tl=tl, Sq=Sq: e.tensor_tensor(
                    out=Sq[:], in0=iota[:],
                    in1=posm[:, tl * 4:(tl + 1) * 4, e_:e_ + 1].to_broadcast([128, 4, CAPS]), op=ALU.is_equal),
                    r=["iota", "posm"], w=[("S_", q)])
                for b in range(4):
                    P.pe(lambda e, b=b, Sq=Sq: e.transpose(self.psBt[:, b * 128:(b + 1) * 128], Sq[:, b, :],
                                                           self.ident_bf[:]),
                         r=[("S_", q), "identb"], w=[("psB", 0)])
                P.act(lambda e, STq=STq: e.copy(out=STq[:].rearrange("p b t -> p (b t)"), in_=self.psBt[:, 0:512]),
                      r=[("psB", 0)], w=[("ST", q)])
                for half in range(2):
                    bk = half
                    for fc in range(4):
                        f = half * 4 + fc
                        for b in range(4):
                            P.pe(lambda e, f=f, fc=fc, b=b, bk=bk, tl=tl, Sq=Sq: e.matmul(
                                psF[bk][:, fc * CAPS:(fc + 1) * CAPS], xtok[:, tl * 4 + b, f * 128:(f + 1) * 128],
                                Sq[:, b, :], start=(b == 0), stop=(b == 3)),
                                r=["xtok", ("S_", q)], w=[("psF", bk)])
                    P.act(lambda e, half=half, bk=bk: e.copy(
                        out=xeT[:, half * 4:(half + 1) * 4, :].rearrange("p a c -> p (a c)"), in_=psF[bk][:]),
                        r=[("psF", bk)], w=[("xeT", half)])
                for half in range(2):
                    for mc in range(4):
                        m = half * 4 + mc
                        for k in range(8):
                            P.pe(lambda e, k=k, m=m, mc=mc: e.matmul(psF[2][:, mc * CAPS:(mc + 1) * CAPS],
                                                                     Wg[:, k, m * 128:(m + 1) * 128], xeT[:, k, :],
                                                                     start=(k == 0), stop=(k == 7)),
                                 r=[("W", 0, j), ("xeT", 0), ("xeT", 1)], w=[("psF", 2)])
                        for k in range(8):
                            P.pe(lambda e, k=k, m=m, mc=mc: e.matmul(psF[3][:, mc * CAPS:(mc + 1) * CAPS],
                                                                     Wu[:, k, m * 128:(m + 1) * 128], xeT[:, k, :],
                                                                     start=(k == 0), stop=(k == 7)),
                                 r=[("W", 1, j), ("xeT", 0), ("xeT", 1)], w=[("psF", 3)])
                    P.act(lambda e: e.activation(out=sgs[:].rearrange("p a c -> p (a c)"), in_=psF[2][:],
                                                 func=AF.Silu), r=[("psF", 2)], w=["sgs"])
                    P.dve(lambda e, half=half: e.tensor_tensor(
                        out=hTs[:, half * 4:(half + 1) * 4, :].rearrange("p a c -> p (a c)"), in0=psF[3][:],
                        in1=sgs[:].rearrange("p a c -> p (a c)"), op=ALU.mult),
                        r=[("psF", 3), "sgs"], w=[("hTs", half)])
                yq = 0
                for n in range(2):
                    bD = 4 + n
                    for k in range(8):
                        P.pe(lambda e, k=k, n=n, bD=bD: e.matmul(psF[bD][:], hTs[:, k, :], Wd[:, k, n * 512:(n + 1) * 512],
                                                                 start=(k == 0), stop=(k == 7)),
                             r=[("hTs", 0), ("hTs", 1), ("W", 2, j)], w=[("psF", bD)])
                    P.act(lambda e, n=n, bD=bD, yq=yq: e.copy(out=yeb[yq][:, n * 512:(n + 1) * 512], in_=psF[bD][:]),
                          r=[("psF", bD)], w=[("yeb", yq, n)])
                for b in range(4):
                    s8 = tl * 4 + b
                    blk = st * 8 + s8
                    gate = Gt[:, blk, e_:e_ + 1]
                    for n in range(2):
                        bS = (0, 1, 6)[scnt["d"] % 3]
                        scnt["d"] += 1
                        P.pe(lambda e, b=b, n=n, bS=bS, STq=STq, yq=yq: e.matmul(
                            psF[bS][:], STq[:, b, :], yeb[yq][:, n * 512:(n + 1) * 512], start=True, stop=True),
                            r=[("ST", q), ("yeb", yq, n)], w=[("psF", bS)])
                        asl = acc[:, s8, n * 512:(n + 1) * 512]
                        if e_ == 0:
                            P.dve(lambda e, bS=bS, asl=asl, gate=gate: e.tensor_scalar(
                                out=asl, in0=psF[bS][:], scalar1=gate, scalar2=None, op0=ALU.mult),
                                r=[("psF", bS), "Gt"], w=[("acc", s8)])
                        else:
                            P.dve(lambda e, bS=bS, asl=asl, gate=gate: e.scalar_tensor_tensor(
                                out=asl, in0=psF[bS][:], scalar=gate, in1=asl, op0=ALU.mult, op1=ALU.add),
                                r=[("psF", bS), "Gt", ("acc", s8)], w=[("acc", s8)])

        def finish(st):
            for s8 in range(8):
                xj = 0
                row0 = st * STT + s8 * 128
                P.dma("sp", lambda e, xj=xj, row0=row0: e.dma_start(out=xm[xj][:], in_=self.XM[row0:row0 + 128, :]),
                      r=[("XM", row0 // TS)], w=[("xm7", xj)], key=f"xm7{xj}")
                P.dve(lambda e, s8=s8, xj=xj: e.tensor_tensor(out=acc[:, s8, :], in0=acc[:, s8, :], in1=xm[xj][:],
                                                              op=ALU.add),
                      r=[("acc", s8), ("xm7", xj)], w=[("acc", s8)])
            accn = [("acc", s8) for s8 in range(8)]
            if not last:
                P.dma("pool", lambda e: e.dma_start(
                    out=self.XA[st * STT:(st + 1) * STT, :].rearrange("(a p) f -> p a f", p=128), in_=acc[:]),
                    r=accn, w=[("XA", 2 * st), ("XA", 2 * st + 1)], key="xast")
            else:
                P.dve(lambda e: e.memset(ss[:], 0.0), w=["ss7"])
                for s8 in range(8):
                    P.act(lambda e, s8=s8: e.activation(out=junk, in_=acc[:, s8, :], func=AF.Square,
                                                        accum_out=ss[:, s8:s8 + 1]),
                          r=[("acc", s8), "ss7"], w=[("hT7", k) for k in range(8)] + ["ss7"])
                P.act(lambda e: e.activation(out=rstd[:], in_=ss[:], func=AF.Ln, scale=1.0 / D, bias=self.epsb[:]),
                      r=["ss7", "epsb"], w=["rstd7"])
                P.act(lambda e: e.activation(out=rstd[:], in_=rstd[:], func=AF.Exp, scale=-0.5),
                      r=["rstd7"], w=["rstd7"])
                for s8 in range(8):
                    P.dve(lambda e, s8=s8: e.scalar_tensor_tensor(
                        out=acc[:, s8, :], in0=acc[:, s8, :], scalar=rstd[:, s8:s8 + 1], in1=fnw[:],
                        op0=ALU.mult, op1=ALU.mult), r=[("acc", s8), "rstd7", "fnw"], w=[("acc", s8)])
                P.dma("pool", lambda e: e.dma_start(
                    out=self.y_out[st * STT:(st + 1) * STT, :].rearrange("(a p) f -> p a f", p=128), in_=acc[:]),
                    r=accn, w=[("Y", st)], key="yst")

        pend = loadw(0)
        for st in range(NST):
            if self.sparse:
                P.dma("sp", lambda e, st=st: e.dma_start(
                    out=xtok[:], in_=self.XNK[st * STT:(st + 1) * STT, :].rearrange("(a p) f -> p a f", p=128)),
                    r=[("XNK", 2 * st), ("XNK", 2 * st + 1)], w=["xtok"], key="xtok")
                positions(st)
            else:
                P.dma("sp", lambda e, st=st: e.dma_start(out=xnT[:], in_=self.XN[:, :, st * STT:(st + 1) * STT]),
                      r=[("XN", 2 * st), ("XN", 2 * st + 1)], w=["xnT7"], key="xn7")
            for e_ in range(NEX):
                j = pend
                nxt = (st, e_ + 1) if e_ + 1 < NEX else ((st + 1, 0) if st + 1 < NST else None)
                if nxt is not None:
                    pend = loadw(nxt[1])
                if self.sparse:
                    expert_sparse(st, e_, j)
                else:
                    expert(st, e_, j)
            finish(st)

def _rope_tables(pos):
    half = 8
    inv_freq = (np.float32(ROPE_THETA) ** (-np.arange(half, dtype=np.float32) * np.float32(2.0) / np.float32(16)))
    inv_freq = inv_freq.astype(np.float32)
    ang = pos.astype(np.float32)[:, None] * inv_freq[None, :]
    cos = np.cos(ang.astype(np.float64)).astype(np.float32)
    sin = np.sin(ang.astype(np.float64)).astype(np.float32)
    C = np.ones((64, pos.shape[0]), np.float32)
    S = np.zeros((64, pos.shape[0]), np.float32)
    C[0:8] = cos.T
    C[8:16] = cos.T
    S[0:8] = -sin.T
    S[8:16] = sin.T
    return np.concatenate([C, C], 0), np.concatenate([S, S], 0)


def _core_consts(core):
    seqlen = NTOK if core < 4 else 2048
    pos = np.arange(NTOK) % seqlen
    C, S = _rope_tables(pos)
    flags = np.ones((128, 2, NCHUNK), np.float32)
    for c in range(NCHUNK):
        if (c * CH) % seqlen == 0:
            flags[:, 0, c] = 0.0
        if ((c + 1) * CH) % seqlen == 0:
            flags[:, 1, c] = 0.0
    kk = np.arange(128)[:, None]
    qq = np.arange(128)[None, :]
    m1 = np.where(kk >= qq, 0.0, MASKNEG)
    m2 = np.where(kk <= qq, 0.0, MASKNEG)
    m1e = np.where((kk >= qq) & (kk >= 64), 0.0, MASKNEG)
    m2e = np.where((kk <= qq) & (kk < 64), 0.0, MASKNEG)
    am = np.zeros((128, 3, 2, 128), np.float32)
    am[:, 0, 0], am[:, 0, 1] = m1, m2
    am[:, 1, 0], am[:, 1, 1] = m1e, m2e
    if core < 4:
        am[:, 2, 0], am[:, 2, 1] = m1, m2
    else:
        am[:, 2, 0], am[:, 2, 1] = m1e, m2e
    return dict(c_ropeC=C, c_ropeS=S, c_flags=flags, c_amask=am.astype(np.float32))


def _shared_consts(inp):
    f32 = np.float32
    w_in = np.asarray(inp["w_in"], f32)
    def rotperm(w):
        w = w.reshape(DEPTH, D, 6, 64)
        o = np.zeros_like(w)
        o[..., 0:8] = w[..., 8:16]
        o[..., 8:16] = w[..., 0:8]
        return o.reshape(DEPTH, D, 384)
    qa = w_in[:, :, 3200:3584]
    ka = w_in[:, :, 3584:3968]
    w_ext = np.concatenate([w_in, rotperm(qa), rotperm(ka)], axis=2)
    def fm(v, n):
        return np.ascontiguousarray(np.asarray(v, f32).reshape(DEPTH, n, 128).transpose(2, 0, 1))
    lbl = np.stack([np.asarray(inp["lb_fwd_logits"], f32), np.asarray(inp["lb_bwd_logits"], f32)], 0)
    lbl = np.ascontiguousarray(lbl.reshape(2, DEPTH, HG_H, 128).transpose(3, 0, 2, 1))
    rst = np.ones((128, TS), f32)
    rst[:, ::CH] = 0.0
    s = np.arange(CH)[:, None]
    t = np.arange(CH)[None, :]
    hmask = np.zeros((CH, 2, CH), f32)
    hmask[:, 0, :] = np.where(s <= t, -1.0, 0.0)
    hmask[:, 1, :] = np.where(s >= t, -1.0, 0.0)
    return dict(
        w_in=np.ascontiguousarray(w_ext),
        w_out=np.asarray(inp["w_out"], f32), w_router=np.asarray(inp["w_router"], f32),
        w_gate=np.asarray(inp["w_gate"], f32), w_up=np.asarray(inp["w_up"], f32),
        w_down=np.asarray(inp["w_down"], f32),
        c_n1=fm(inp["norm1_w"], 8), c_n2=fm(inp["norm2_w"], 8), c_hgw=fm(inp["hgrn_norm_w"], HG_H),
        c_fnw=np.asarray(inp["final_norm_w"], f32).reshape(1, D),
        c_lbl=lbl, c_ident=np.eye(128, dtype=f32), c_rst=rst, c_hmask=hmask,
        c_tri=(np.arange(128)[:, None] < np.arange(128)[None, :]).astype(f32),
        c_iota=np.ascontiguousarray(np.broadcast_to(np.arange(128, dtype=f32)[None, None, :], (128, 4, 128))),
        c_n2row=np.asarray(inp["norm2_w"], f32),
        c_gsum=(np.arange(64)[:, None] % NE == np.arange(64)[None, :] % NE).astype(f32),
        c_dsel=(np.arange(64)[:, None] == np.arange(NE)[None, :]).astype(f32),
    )


def make_in_maps(inp):
    shared = _shared_consts(inp)
    xp = np.asarray(inp["x_prompt"], np.float32)
    xsm = np.asarray(inp["x_sample"], np.float32)
    maps = []
    for c in range(NCORES):
        m = dict(shared)
        if c < 4:
            m["x"] = np.ascontiguousarray(xp[c])
        else:
            m["x"] = np.ascontiguousarray(xsm[(c - 4) * 4:(c - 3) * 4].reshape(NTOK, D))
        m.update(_core_consts(c))
        maps.append(m)
    return maps


def kernel(**inputs):
    b = Builder()
    nc = b.build()
    maps = make_in_maps(inputs)
    res = run_bass_kernel_spmd(nc, maps, core_ids=list(range(NCORES)))
    ys = [np.asarray(r["y"], np.float32) for r in res.results]
    y_prompt = np.stack(ys[0:4], 0)
    y_sample = np.concatenate([y.reshape(4, 2048, D) for y in ys[4:8]], 0)
    return (y_prompt, y_sample)
```
